# Optimizing a Trainium2 kernel written in Bass

```python
import math
import jax, jax.numpy as jnp
from jax import lax
import numpy as np

D_MODEL = 1024
BATCH = 4
SEQ = 8192
DEPTH = 1

MLA_HEADS = 8
MLA_Q_RANK = 384
MLA_KV_RANK = 256
MLA_NOPE_DIM = 64
MLA_ROPE_DIM = 32
MLA_V_DIM = 64
MLA_QK_DIM = MLA_NOPE_DIM + MLA_ROPE_DIM
FOX_HEADS = 8
FOX_HEAD_DIM = 64
FOX_WIDTH = FOX_HEADS * FOX_HEAD_DIM
MIX_WIDTH = MLA_HEADS * MLA_V_DIM + FOX_WIDTH
ROPE_THETA = 10000.0
BLOCK_Q = 128
IN_SPLITS = (MLA_Q_RANK, MLA_KV_RANK, MLA_ROPE_DIM, FOX_WIDTH, FOX_WIDTH, FOX_WIDTH, FOX_HEADS)
IN_WIDTH = sum(IN_SPLITS)
PEER_HEADS = 8
N_KEYS = 128
N_EXPERTS = N_KEYS * N_KEYS
PEER_QDIM = 256
PEER_HALF = PEER_QDIM // 2
PEER_TOPK = 16
PEER_CHUNK = 64
EPS = 1e-6

kernel_name = "hybrid_mla_fox_peer_adaln_layer"


def _rms_norm(x, g):
    xf = x.astype(jnp.float32)
    y = xf * lax.rsqrt(jnp.mean(xf * xf, axis=-1, keepdims=True) + EPS)
    return (y * g.astype(jnp.float32)).astype(x.dtype)


def _modulate(x, g, shift, scale):
    return _rms_norm(x, g) * (1.0 + scale) + shift


def _rope(x, ang):
    half = x.shape[-1] // 2
    x1, x2 = x[..., :half], x[..., half:]
    cos = jnp.cos(ang).astype(x.dtype)
    sin = jnp.sin(ang).astype(x.dtype)
    return jnp.concatenate([x1 * cos - x2 * sin, x2 * cos + x1 * sin], axis=-1)


def _causal_block_attention(q, k, v, scale, fcum=None):
    b, h, s, dk = q.shape
    dv = v.shape[-1]
    nblk = s // BLOCK_Q
    qb = q.reshape(b, h, nblk, BLOCK_Q, dk).transpose(2, 0, 1, 3, 4)
    kf = k.astype(jnp.float32)
    vf = v.astype(jnp.float32)
    key_pos = jnp.arange(s)
    blk_ids = jnp.arange(nblk)

    def scores(q_i):
        return jnp.einsum("bhqd,bhkd->bhqk", q_i.astype(jnp.float32), kf) * scale

    def finish(i, sc):
        q_pos = i * BLOCK_Q + jnp.arange(BLOCK_Q)
        sc = jnp.where(key_pos[None, :] <= q_pos[:, None], sc, -jnp.inf)
        p = jax.nn.softmax(sc, axis=-1)
        return jnp.einsum("bhqk,bhkd->bhqd", p, vf)

    if fcum is None:
        out = lax.map(lambda a: finish(a[0], scores(a[1])), (blk_ids, qb))
    else:
        fb = fcum.reshape(b, h, nblk, BLOCK_Q).transpose(2, 0, 1, 3)

        def blk(a):
            i, q_i, f_i = a
            sc = scores(q_i) + f_i[..., None] - fcum[:, :, None, :]
            return finish(i, sc)

        out = lax.map(blk, (blk_ids, qb, fb))
    return out.transpose(1, 2, 0, 3, 4).reshape(b, h, s, dv).astype(v.dtype)


def _peer(h, w_pq, sub_keys, peer_u, peer_v):
    b, s, d = h.shape
    qp = (h @ w_pq).astype(jnp.float32).reshape(b, s, PEER_HEADS, 2, PEER_HALF)
    sc = jnp.einsum("bshpd,hpnd->bshpn", qp, sub_keys.astype(jnp.float32))
    s1, i1 = lax.top_k(sc[..., 0, :], PEER_TOPK)
    s2, i2 = lax.top_k(sc[..., 1, :], PEER_TOPK)
    cand = (s1[..., :, None] + s2[..., None, :]).reshape(b, s, PEER_HEADS, PEER_TOPK * PEER_TOPK)
    top_s, top_c = lax.top_k(cand, PEER_TOPK)
    e1 = jnp.take_along_axis(i1, top_c // PEER_TOPK, axis=-1)
    e2 = jnp.take_along_axis(i2, top_c % PEER_TOPK, axis=-1)
    expert = e1 * N_KEYS + e2
    gate = jax.nn.softmax(top_s, axis=-1)
    nc = s // PEER_CHUNK

    def to_chunks(a):
        return a.reshape((b, nc, PEER_CHUNK) + a.shape[2:]).swapaxes(0, 1)

    def chunk(args):
        h_c, e_c, g_c = args
        u = jnp.take(peer_u, e_c, axis=0)
        a = jnp.einsum("bcd,bchkd->bchk", h_c.astype(jnp.float32), u.astype(jnp.float32))
        w = g_c * jax.nn.gelu(a)
        vv = jnp.take(peer_v, e_c, axis=0)
        return jnp.einsum("bchk,bchkd->bcd", w, vv.astype(jnp.float32))

    y = lax.map(chunk, (to_chunks(h), to_chunks(expert), to_chunks(gate)))
    return y.swapaxes(0, 1).reshape(b, s, d).astype(h.dtype)


def setup_inputs(seed: int = 0) -> dict:
    key = jax.random.key(seed)
    ks = jax.random.split(key, 24)
    f32 = jnp.float32
    D = D_MODEL

    def nrm(k, shape, scale):
        return jax.random.normal(k, shape, f32) * scale

    def gain(k, n):
        return 1.0 + 0.02 * jax.random.normal(k, (n,), f32)

    x = jax.random.normal(ks[0], (BATCH, SEQ, D), f32)
    c = jax.random.normal(ks[1], (BATCH, D), f32)
    offset = jax.random.randint(ks[2], (BATCH, 1), 0, 1024, dtype=jnp.int32)
    positions = (offset + jnp.arange(SEQ, dtype=jnp.int32)[None, :]).astype(jnp.int32)
    return {
        "x": x,
        "c": c,
        "positions": positions,
        "w_ada": nrm(ks[3], (D, 6 * D), 0.5 * D ** -0.5),
        "b_ada": nrm(ks[4], (6 * D,), 0.01),
        "norm1_g": gain(ks[5], D),
        "w_in": nrm(ks[6], (D, IN_WIDTH), D ** -0.5),
        "mla_qa_g": gain(ks[7], MLA_Q_RANK),
        "mla_kva_g": gain(ks[8], MLA_KV_RANK),
        "w_uq": nrm(ks[9], (MLA_Q_RANK, MLA_HEADS * MLA_QK_DIM), MLA_Q_RANK ** -0.5),
        "w_ukv": nrm(ks[10], (MLA_KV_RANK, MLA_HEADS * (MLA_NOPE_DIM + MLA_V_DIM)), MLA_KV_RANK ** -0.5),
        "mla_q_g": gain(ks[11], MLA_QK_DIM),
        "mla_k_g": gain(ks[12], MLA_QK_DIM),
        "fox_q_g": gain(ks[13], FOX_HEAD_DIM),
        "fox_k_g": gain(ks[14], FOX_HEAD_DIM),
        "b_f": 2.0 + 0.5 * jax.random.normal(ks[15], (FOX_HEADS,), f32),
        "w_o": nrm(ks[16], (MIX_WIDTH, D), MIX_WIDTH ** -0.5),
        "norm2_g": gain(ks[17], D),
        "w_pq": nrm(ks[18], (D, PEER_HEADS * PEER_QDIM), D ** -0.5),
        "sub_keys": nrm(ks[19], (PEER_HEADS, 2, N_KEYS, PEER_HALF), PEER_HALF ** -0.5),
        "peer_u": nrm(ks[20], (N_EXPERTS, D), D ** -0.5),
        "peer_v": nrm(ks[21], (N_EXPERTS, D), PEER_HEADS ** -0.5),
    }


def reference(x, c, positions, w_ada, b_ada, norm1_g, w_in, mla_qa_g, mla_kva_g, w_uq, w_ukv,
              mla_q_g, mla_k_g, fox_q_g, fox_k_g, b_f, w_o, norm2_g, w_pq, sub_keys, peer_u, peer_v):
    dt = x.dtype
    b, s, d = x.shape
    mod = jax.nn.silu(c.astype(jnp.float32)) @ w_ada.astype(jnp.float32) + b_ada.astype(jnp.float32)
    mod = mod.reshape(b, 6, 1, d).astype(dt)
    shift1, scale1, gate1 = mod[:, 0], mod[:, 1], mod[:, 2]
    shift2, scale2, gate2 = mod[:, 3], mod[:, 4], mod[:, 5]
    inv_freq = ROPE_THETA ** (-jnp.arange(0, MLA_ROPE_DIM, 2, dtype=jnp.float32) / MLA_ROPE_DIM)
    ang = (positions.astype(jnp.float32)[..., None] * inv_freq)[:, :, None, :]

    for _ in range(DEPTH):
        h = _modulate(x, norm1_g, shift1, scale1)
        proj = h @ w_in
        cq, ckv, kpe, fq, fk, fv, fl = jnp.split(proj, list(np.cumsum(IN_SPLITS)[:-1]), axis=-1)

        q_a = (_rms_norm(cq, mla_qa_g) @ w_uq).reshape(b, s, MLA_HEADS, MLA_QK_DIM)
        kv_a = (_rms_norm(ckv, mla_kva_g) @ w_ukv).reshape(b, s, MLA_HEADS, MLA_NOPE_DIM + MLA_V_DIM)
        k_nope, v_a = kv_a[..., :MLA_NOPE_DIM], kv_a[..., MLA_NOPE_DIM:]
        k_pe = jnp.broadcast_to(kpe[:, :, None, :], (b, s, MLA_HEADS, MLA_ROPE_DIM))
        k_a = jnp.concatenate([k_nope, k_pe], axis=-1)
        q_a = _rms_norm(q_a, mla_q_g)
        k_a = _rms_norm(k_a, mla_k_g)
        q_a = jnp.concatenate([q_a[..., :MLA_NOPE_DIM], _rope(q_a[..., MLA_NOPE_DIM:], ang)], axis=-1)
        k_a = jnp.concatenate([k_a[..., :MLA_NOPE_DIM], _rope(k_a[..., MLA_NOPE_DIM:], ang)], axis=-1)
        o_a = _causal_block_attention(q_a.transpose(0, 2, 1, 3), k_a.transpose(0, 2, 1, 3),
                                      v_a.transpose(0, 2, 1, 3), MLA_QK_DIM ** -0.5)

        q_b = _rms_norm(fq.reshape(b, s, FOX_HEADS, FOX_HEAD_DIM), fox_q_g)
        k_b = _rms_norm(fk.reshape(b, s, FOX_HEADS, FOX_HEAD_DIM), fox_k_g)
        v_b = fv.reshape(b, s, FOX_HEADS, FOX_HEAD_DIM)
        log_f = jax.nn.log_sigmoid(fl.astype(jnp.float32) + b_f.astype(jnp.float32))
        fcum = jnp.cumsum(log_f, axis=1).transpose(0, 2, 1)
        o_b = _causal_block_attention(q_b.transpose(0, 2, 1, 3), k_b.transpose(0, 2, 1, 3),
                                      v_b.transpose(0, 2, 1, 3), FOX_HEAD_DIM ** -0.5, fcum)

        o = jnp.concatenate([o_a, o_b], axis=1).transpose(0, 2, 1, 3).reshape(b, s, MIX_WIDTH)
        x = x + gate1 * (o @ w_o)

        h2 = _modulate(x, norm2_g, shift2, scale2)
        x = x + gate2 * _peer(h2, w_pq, sub_keys, peer_u, peer_v)
    return x
```

```python
import numpy as np
from contextlib import ExitStack
import ml_dtypes
import concourse.bass as bass
import concourse.mybir as mybir
from concourse.bass_utils import run_bass_kernel_spmd

F32 = mybir.dt.float32
BF16 = mybir.dt.bfloat16
I32 = mybir.dt.int32
U32 = mybir.dt.uint32
AF = mybir.ActivationFunctionType
ALU = mybir.AluOpType
AX = mybir.AxisListType

D = 1024
S = 8192
NT = 64
NO = 32
SO = 4096
EPS = 1e-6
MAGIC = 12582912.0
TWO_PI = 6.283185307179586
NEG = -30000.0

STOP_AFTER = None
DEBUG = False


PSUM_KEYS = {"S0", "S1", "S2", "S3", "Q0", "Q1", "P4", "P4b", "kvp", "tpB", "tpK", "ops0", "ops1"}


class Tracker:
    L = 4000

    def __init__(self, nc, es):
        self.nc = nc
        self.es = es
        self.eng = {"pe": nc.tensor, "act": nc.scalar, "dve": nc.vector, "pool": nc.gpsimd, "sp": nc.sync}
        self.cnt = {e: 0 for e in self.eng}
        self.sems = {e: [] for e in self.eng}
        self.dsem = {}
        self.dval = {}
        self.waited = {}
        self.lastw = {}
        self.readers = {}

    def _csem(self, e, k):
        i = k // self.L
        while len(self.sems[e]) <= i:
            self.sems[e].append(self.es.enter_context(self.nc.semaphore(f"p_{e}_{len(self.sems[e])}")))
        return self.sems[e][i]

    def _wait(self, eng, dep):
        if dep[0] == "c":
            _, E, k = dep
            if eng == "pe" and E == "pe":
                return
            key = (eng, "c", E)
            if self.waited.get(key, -1) >= k:
                return
            self.waited[key] = k
            self.eng[eng].wait_ge(self._csem(E, k), k % self.L + 1)
        else:
            _, sid, val = dep
            key = (eng, "d", sid)
            if self.waited.get(key, -1) >= val:
                return
            self.waited[key] = val
            self.eng[eng].wait_ge(self.dsem[sid], val)

    def _deps(self, eng, reads, writes):
        deps = []
        for r in reads:
            if r in self.lastw:
                deps.append(self.lastw[r])
            if r in PSUM_KEYS:
                deps.extend(v for k_, v in self.readers.get(r, {}).items() if k_ != ("c", eng))
        for w in writes:
            if w in self.lastw:
                deps.append(self.lastw[w])
            deps.extend(self.readers.get(w, {}).values())
        for d in deps:
            self._wait(eng, d)

    def _record(self, tok, rkey, reads, writes):
        for r in reads:
            self.readers.setdefault(r, {})[rkey] = tok
        for w in writes:
            self.lastw[w] = tok
            self.readers[w] = {}

    def op(self, e, fn, reads=(), writes=()):
        self._deps(e, reads, writes)
        inst = fn(self.eng[e])
        k = self.cnt[e]
        inst.then_inc(self._csem(e, k), 1)
        self.cnt[e] = k + 1
        self._record(("c", e, k), ("c", e), reads, writes)
        return inst

    def dma(self, q, out, in_, reads=(), writes=(), key=None, **kw):
        self._deps(q, reads, writes)
        if key not in self.dsem:
            self.dsem[key] = self.es.enter_context(self.nc.semaphore(f"d_{key}"))
            self.dval[key] = 0
        self.dval[key] += 16
        self.eng[q].dma_start(out=out, in_=in_, **kw).then_inc(self.dsem[key], 16)
        self._record(("d", key, self.dval[key]), ("d", key), reads, writes)

    def drain(self):
        for e in self.eng:
            for E in self.eng:
                if E != e and self.cnt[E] > 0:
                    self._wait(e, ("c", E, self.cnt[E] - 1))
            for sid, v in self.dval.items():
                self._wait(e, ("d", sid, v))


def build_program():
    nc = bass.Bass("TRN2", target_bir_lowering=False)

    def din(name, shape, dt=F32):
        return nc.dram_tensor(name, list(shape), dt, kind="ExternalInput").ap()

    def dscr(name, shape, dt):
        return nc.dram_tensor(name, list(shape), dt, kind=("ExternalOutput" if DEBUG else "Internal")).ap()

    x_all = din("x_all", [S, D])
    x_own = din("x_own", [SO, D])
    cT = din("cT", [128, 8])
    pos_all = din("pos_all", [128, NT], I32)
    pos_own = din("pos_own", [128, NO], I32)
    w_ada = din("w_ada", [D, 6 * D])
    bada_fm = din("bada_fm", [128, 48])
    bada_row = din("bada_row", [1, 6 * D])
    g1_fm = din("g1_fm", [128, 8])
    g2_fm = din("g2_fm", [128, 8])
    w_in = din("w_in", [D, 2216])
    gqa_fm = din("gqa_fm", [128, 3])
    gkva_fm = din("gkva_fm", [128, 2])
    w_uq = din("w_uq", [384, 768])
    w_ukv = din("w_ukv", [256, 1024])
    gq_row = din("gq_row", [1, 96])
    gk_row = din("gk_row", [1, 96])
    gfq_row = din("gfq_row", [1, 64])
    gfk_row = din("gfk_row", [1, 64])
    bf_col = din("bf_col", [8, 1])
    w_o = din("w_o", [D, D])
    w_pq = din("w_pq", [D, 2048])
    skT = din("skT", [128, 16 * 128])
    U_arr = din("U_arr", [16384, 1024])
    V_in = din("V_in", [16384, 1024])
    c_ident_bf = din("c_ident_bf", [128, 128], BF16)
    c_ident_f = din("c_ident_f", [128, 128])
    c_maskA = din("c_maskA", [128, 128], BF16)
    c_maskB = din("c_maskB", [128, 128], BF16)
    c_invf = din("c_invf", [128, 16])
    c_iota = din("c_iota", [128, 128])
    c_par = din("c_par", [128, 1])
    out_own = nc.dram_tensor("out_own", [SO, D], F32, kind="ExternalOutput").ap()

    KT_mla = dscr("KT_mla", [8, 96, S], BF16)
    QT_mla = dscr("QT_mla", [8, 96, SO], BF16)
    V_mla = dscr("V_mla", [NT, 128, 8, 65], BF16)
    KTF_main = dscr("KTF_main", [8, 64, S], BF16)
    KTF_aug = dscr("KTF_aug", [8, 6, S], BF16)
    QTF_main = dscr("QTF_main", [8, 64, SO], BF16)
    QTF_aug = dscr("QTF_aug", [8, 6, SO], BF16)
    V_fox = dscr("V_fox", [NT, 128, 8, 65], BF16)
    OT = dscr("OT", [D, SO], BF16)
    X1 = dscr("X1", [SO, D], F32)
    H2T = dscr("H2T", [128, 8, SO], BF16)
    ET = dscr("ET", [3, 128, SO], F32)
    G1S = dscr("G1S", [128, D], F32)
    Ubf = dscr("Ubf", [16384, 1024], BF16)
    Vbf = dscr("Vbf", [16384, 1024], BF16)

    def brow(ap2d, lo, n):
        return bass.AP(ap2d.tensor, lo, [[0, 128], [1, n]])

    with ExitStack() as es:
        T = Tracker(nc, es)
        _n = [0]

        def sb(es_, shape, dt, name=None):
            _n[0] += 1
            return es_.enter_context(nc.sbuf_tensor(name or f"t{_n[0]}", list(shape), dt))

        P4 = es.enter_context(nc.psum_tensor("P4", [128, 2048], F32))
        Q4 = es.enter_context(nc.psum_tensor("Q4", [128, 2048], F32))
        SB_ = [Q4[:, i * 512:(i + 1) * 512] for i in range(4)]

        def bfv(ap):
            return ap.bitcast(BF16).rearrange("p (c t) -> p c t", t=128)

        ident_bf = sb(es, [128, 128], BF16, "ident_bf")
        ident_f = sb(es, [128, 128], F32, "ident_f")
        maskA = sb(es, [128, 128], BF16, "maskA")
        maskB = sb(es, [128, 128], BF16, "maskB")
        iota = sb(es, [128, 128], F32, "iota")
        parc = sb(es, [128, 1], F32, "parc")
        A1 = sb(es, [128, 8], F32, "A1")
        B1 = sb(es, [128, 8], F32, "B1")
        A2 = sb(es, [128, 8], F32, "A2")
        B2 = sb(es, [128, 8], F32, "B2")
        gate2 = sb(es, [128, D], F32, "gate2")
        mhalf = sb(es, [128, 8], F32, "mhalf")
        ones_f = sb(es, [128, 64], F32, "ones_f")

        ld = lambda o, i, k, **kw: T.dma("sp", o, i, writes=[k], key="ld_" + k, **kw)
        ld(ident_bf[:], c_ident_bf[:, :], "ident_bf")
        ld(ident_f[:], c_ident_f[:, :], "ident_f")
        ld(maskA[:], c_maskA[:, :], "maskA")
        ld(maskB[:], c_maskB[:, :], "maskB")
        ld(iota[:], c_iota[:, :], "iota")
        ld(parc[:], c_par[:, :], "parc")
        T.op("dve", lambda e: e.memset(mhalf[:], -0.5), writes=["mhalf"])
        T.op("dve", lambda e: e.memset(ones_f[:], 1.0), writes=["ones_f"])

        cast_jobs = []
        for i in range(16):
            cast_jobs.append((Ubf[i * 1024:(i + 1) * 1024, :], U_arr[i * 1024:(i + 1) * 1024, :]))
        for i in range(16):
            cast_jobs.append((Vbf[i * 1024:(i + 1) * 1024, :], V_in[i * 1024:(i + 1) * 1024, :]))

        def issue_cast():
            if cast_jobs:
                o, i = cast_jobs.pop(0)
                T.dma("pool", o, i, key="uvcast")

        def rsqrt_mean(ss_ap, key_ss, out_ap, key_out, n, tmp_ap, key_tmp):
            m = ss_ap.shape[-1]
            T.op("dve", lambda e: e.tensor_scalar(out=tmp_ap, in0=ss_ap, scalar1=1.0 / n, scalar2=EPS,
                                                  op0=ALU.mult, op1=ALU.add), reads=[key_ss], writes=[key_tmp])
            T.op("pool", lambda e: e.tensor_tensor(out=out_ap, in0=tmp_ap, in1=mhalf[:, 0:m], op=ALU.pow),
                 reads=[key_tmp, "mhalf"], writes=[key_out])

        pfl = es.enter_context(ExitStack())
        FLT = sb(pfl, [8, S], F32, "FLT")
        with ExitStack() as pb:
            w_in_sb = sb(pb, [128, 8, 2216], BF16, "w_in_sb")
            w_uq_sb = sb(pb, [128, 3, 768], BF16, "w_uq_sb")
            w_ukv_sb = sb(pb, [128, 2, 1024], BF16, "w_ukv_sb")
            for c in range(8):
                T.dma("pool", w_in_sb[:, c, :], w_in[c * 128:(c + 1) * 128, :], writes=["w_in_sb"], key="ld_w_in",
                      max_dma_last_dim=4096)
            T.dma("pool", w_uq_sb[:], w_uq.rearrange("(c p) n -> p c n", p=128), writes=["w_uq_sb"], key="ld_w_uq")
            T.dma("pool", w_ukv_sb[:], w_ukv.rearrange("(c p) n -> p c n", p=128), writes=["w_ukv_sb"], key="ld_w_ukv")

            gqa = sb(pb, [128, 3], F32, "gqa")
            gkva = sb(pb, [128, 2], F32, "gkva")
            gq_bc = sb(pb, [128, 96], F32, "gq_bc")
            gk_bc = sb(pb, [128, 96], F32, "gk_bc")
            gfq_bc = sb(pb, [128, 64], F32, "gfq_bc")
            gfk_bc = sb(pb, [128, 64], F32, "gfk_bc")
            ld(gqa[:], gqa_fm[:, :], "gqa")
            ld(gkva[:], gkva_fm[:, :], "gkva")
            ld(gq_bc[:], brow(gq_row, 0, 96), "gq_bc")
            ld(gk_bc[:], brow(gk_row, 0, 96), "gk_bc")
            ld(gfq_bc[:], brow(gfq_row, 0, 64), "gfq_bc")
            ld(gfk_bc[:], brow(gfk_row, 0, 64), "gfk_bc")
            T.op("dve", lambda e: e.tensor_scalar(out=gq_bc[:], in0=gq_bc[:], scalar1=96.0 ** -0.5, scalar2=None,
                                                  op0=ALU.mult), reads=["gq_bc"], writes=["gq_bc"])
            T.op("dve", lambda e: e.tensor_scalar(out=gfq_bc[:], in0=gfq_bc[:], scalar1=0.125, scalar2=None,
                                                  op0=ALU.mult), reads=["gfq_bc"], writes=["gfq_bc"])

            with ExitStack() as pa:
                c_sb = sb(pa, [128, 8], F32, "c_sb")
                sc = sb(pa, [128, 8], F32, "sc")
                sc_rep = sb(pa, [128, 8, 128], F32, "sc_rep")
                wa = [sb(pa, [128, 8, 1024], F32, f"wa{i}") for i in range(2)]
                bfm = sb(pa, [128, 48], F32, "bfm")
                modfm = sb(pa, [128, 48], F32, "modfm")
                g1s = sb(pa, [128, 8], F32, "g1s")
                g2s = sb(pa, [128, 8], F32, "g2s")
                bg = sb(pa, [128, 1024], F32, "bg")
                gate1 = sb(pa, [128, D], F32, "gate1")
                ld(c_sb[:], cT[:, :], "c_sb")
                ld(bfm[:], bada_fm[:, :], "bfm")
                ld(g1s[:], g1_fm[:, :], "g1s")
                ld(g2s[:], g2_fm[:, :], "g2s")
                T.op("act", lambda e: e.activation(out=sc[:], in_=c_sb[:], func=AF.Silu), reads=["c_sb"], writes=["sc"])
                T.op("dve", lambda e: e.tensor_copy(out=sc_rep[:], in_=sc[:].unsqueeze(2).to_broadcast([128, 8, 128])),
                     reads=["sc"], writes=["sc_rep"])
                mod_ps = SB_[0]
                w_ada_v = w_ada.rearrange("(c p) n -> p c n", p=128)
                for v in range(6):
                    wv = wa[v % 2]
                    wk = f"wa{v % 2}"
                    T.dma("sp", wv[:], w_ada_v[:, :, v * 1024:(v + 1) * 1024], writes=[wk], key="ld_" + wk)
                    if v in (2, 5):
                        gdst, gk = (gate1, "gate1") if v == 2 else (gate2, "gate2")
                        T.dma("sp", bg[:], brow(bada_row, v * 1024, 1024), writes=["bg"], key="ld_bg")
                        for half in range(2):
                            gp = SB_[1 + half]
                            for kc in range(8):
                                T.op("pe", lambda e, kc=kc, gp=gp, half=half, wv=wv: e.matmul(
                                    gp[:, :], lhsT=sc_rep[:, kc, :], rhs=wv[:, kc, half * 512:(half + 1) * 512],
                                    start=(kc == 0), stop=(kc == 7)), reads=["sc_rep", wk], writes=[f"S{1 + half}"])
                            T.op("dve", lambda e, gp=gp, half=half, gdst=gdst: e.tensor_tensor(
                                out=gdst[:, half * 512:(half + 1) * 512], in0=gp[:, :], in1=bg[:, half * 512:(half + 1) * 512],
                                op=ALU.add), reads=[f"S{1 + half}", "bg"], writes=[gk])
                    else:
                        for oc in range(8):
                            col = v * 8 + oc
                            for kc in range(8):
                                T.op("pe", lambda e, kc=kc, oc=oc, col=col, wv=wv: e.matmul(
                                    mod_ps[:, col:col + 1], lhsT=wv[:, kc, oc * 128:(oc + 1) * 128], rhs=sc[:, kc:kc + 1],
                                    start=(kc == 0), stop=(kc == 7)), reads=["sc", wk], writes=["S0"])
                for (c0_, c1_) in ((0, 16), (24, 40)):
                    T.op("dve", lambda e: e.tensor_tensor(out=modfm[:, c0_:c1_], in0=mod_ps[:, c0_:c1_], in1=bfm[:, c0_:c1_], op=ALU.add),
                         reads=["S0", "bfm"], writes=["modfm"])
                T.op("dve", lambda e: e.scalar_tensor_tensor(out=A1[:], in0=modfm[:, 8:16], scalar=1.0, in1=g1s[:],
                                                             op0=ALU.add, op1=ALU.mult), reads=["modfm", "g1s"], writes=["A1"])
                T.op("dve", lambda e: e.tensor_copy(out=B1[:], in_=modfm[:, 0:8]), reads=["modfm"], writes=["B1"])
                T.op("dve", lambda e: e.scalar_tensor_tensor(out=A2[:], in0=modfm[:, 32:40], scalar=1.0, in1=g2s[:],
                                                             op0=ALU.add, op1=ALU.mult), reads=["modfm", "g2s"], writes=["A2"])
                T.op("dve", lambda e: e.tensor_copy(out=B2[:], in_=modfm[:, 24:32]), reads=["modfm"], writes=["B2"])
                T.dma("sp", G1S[:, :], gate1[:], reads=["gate1"], key="st_gate1")
                T.drain()

            cos_all = sb(pb, [128, NT, 16], F32, "cos_all")
            sin_all = sb(pb, [128, NT, 16], F32, "sin_all")
            cos_own = sb(pb, [128, NO, 16], F32, "cos_own")
            sin_own = sb(pb, [128, NO, 16], F32, "sin_own")
            with ExitStack() as pr:
                invf = sb(pr, [128, 16], F32, "invf")
                ld(invf[:], c_invf[:, :], "invf")
                for (pos_d, n, ctab, stab, nm) in ((pos_all, NT, cos_all, sin_all, "all"), (pos_own, NO, cos_own, sin_own, "own")):
                    pi = sb(pr, [128, n], I32, "pi_" + nm)
                    pf = sb(pr, [128, n], F32, "pf_" + nm)
                    ang = sb(pr, [128, n, 16], F32, "ang_" + nm)
                    t1 = sb(pr, [128, n, 16], F32, "t1_" + nm)
                    t2 = sb(pr, [128, n, 16], F32, "t2_" + nm)
                    ld(pi[:], pos_d[:, :], "pi_" + nm)
                    T.op("dve", lambda e: e.tensor_copy(out=pf[:], in_=pi[:]), reads=["pi_" + nm], writes=["pf_" + nm])
                    T.op("dve", lambda e: e.tensor_tensor(out=ang[:], in0=pf[:].unsqueeze(2).to_broadcast([128, n, 16]),
                                                          in1=invf[:].unsqueeze(1).to_broadcast([128, n, 16]), op=ALU.mult),
                         reads=["pf_" + nm, "invf"], writes=["ang_" + nm])
                    for (tab, tk, shift) in ((stab, "sin_" + nm, 0.0), (ctab, "cos_" + nm, TWO_PI / 4)):
                        T.op("dve", lambda e: e.tensor_scalar(out=t1[:], in0=ang[:], scalar1=shift, scalar2=1.0 / TWO_PI,
                                                              op0=ALU.add, op1=ALU.mult), reads=["ang_" + nm], writes=["t1_" + nm])
                        T.op("dve", lambda e: e.tensor_scalar(out=t2[:], in0=t1[:], scalar1=MAGIC, scalar2=None, op0=ALU.add),
                             reads=["t1_" + nm], writes=["t2_" + nm])
                        T.op("dve", lambda e: e.tensor_scalar(out=t1[:], in0=t2[:], scalar1=-MAGIC, scalar2=-TWO_PI,
                                                              op0=ALU.add, op1=ALU.mult), reads=["t2_" + nm], writes=["t1_" + nm])
                        T.op("dve", lambda e: e.scalar_tensor_tensor(out=t2[:], in0=ang[:], scalar=shift, in1=t1[:],
                                                                     op0=ALU.add, op1=ALU.add), reads=["ang_" + nm, "t1_" + nm],
                             writes=["t2_" + nm])
                        T.op("dve", lambda e: e.tensor_scalar(out=t2[:], in0=t2[:], scalar1=3.1415925, scalar2=-3.1415925,
                                                              op0=ALU.min, op1=ALU.max), reads=["t2_" + nm], writes=["t2_" + nm])
                        T.op("act", lambda e, tab=tab: e.activation(out=tab[:], in_=t2[:], func=AF.Sin),
                             reads=["t2_" + nm], writes=[tk])
                T.drain()

            ones_bf = sb(pb, [1, 128], BF16, "ones_bf")
            brow = sb(pb, [1, 2216], BF16, "brow")
            B1b = sb(pb, [128, 8], BF16, "B1b")
            T.op("dve", lambda e: e.memset(ones_bf[:], 1.0), writes=["ones_bf"])
            T.op("dve", lambda e: e.tensor_copy(out=B1b[:], in_=B1[:]), reads=["B1"], writes=["B1b"])
            for c0_ in range(0, 2216, 512):
                c1_ = min(2216, c0_ + 512)
                for c in range(8):
                    T.op("pe", lambda e: e.matmul(SB_[0][0:1, 0:c1_ - c0_], lhsT=B1b[:, c:c + 1], rhs=w_in_sb[:, c, c0_:c1_],
                                                  start=(c == 0), stop=(c == 7)), reads=["B1b", "w_in_sb"], writes=["S0"])
                T.op("act", lambda e: e.activation(out=brow[0:1, c0_:c1_], in_=SB_[0][0:1, 0:c1_ - c0_], func=AF.Copy),
                     reads=["S0"], writes=["brow"])
            for c in range(8):
                T.op("dve", lambda e: e.tensor_scalar(out=w_in_sb[:, c, :], in0=w_in_sb[:, c, :], scalar1=A1[:, c:c + 1], scalar2=None,
                                                      op0=ALU.mult), reads=["w_in_sb", "A1"], writes=["w_in_sb"])

            xa = [sb(pb, [128, D], F32, f"xa{i}") for i in range(2)]
            xn = sb(pb, [128, D], BF16, "xn")
            hT = sb(pb, [128, 8, 128], BF16, "hT")

            def statset(tag, jw):
                return dict(junk=sb(pb, [128, jw], F32, "junk_" + tag), st=sb(pb, [128, 8], F32, "st_" + tag),
                            st2=sb(pb, [128, 8], F32, "st2_" + tag), rs=sb(pb, [128, 8], F32, "rs_" + tag), tag=tag)
            SX = statset("x", D)
            SC = statset("c", 384)
            SK = statset("k", 1024)
            SF = statset("f", 512)
            junk_p = sb(pb, [128, 32], F32, "junk_p")
            sm = sb(pb, [128, 8], F32, "sm")
            cs = sb(pb, [128, 384], BF16, "cs")
            cT_sb = sb(pb, [128, 3, 128], BF16, "cT_sb")
            kpg = sb(pb, [128, 32], F32, "kpg")
            kr = sb(pb, [128, 32], F32, "kr")
            rtk = sb(pb, [128, 8, 64], F32, "rtk")
            rtq = sb(pb, [128, 8, 64], F32, "rtq")
            qg = sb(pb, [128, 8, 96], F32, "qg")
            qgf = sb(pb, [128, 8, 64], F32, "qgf")
            Kt = sb(pb, [128, 8, 96], BF16, "Kt")
            KTt = sb(pb, [128, 8, 128], BF16, "KTt")
            Vt = [sb(pb, [128, 8, 65], BF16, f"Vt{i}") for i in range(2)]
            Ft = sb(pb, [128, 8, 64], BF16, "Ft")
            FTt = sb(pb, [128, 4, 128], BF16, "FTt")
            for i in range(2):
                T.op("dve", lambda e, i=i: e.memset(Vt[i][:], 1.0), writes=[f"Vt{i}"])

            tp = bfv(SB_[0][:, :])
            pj = [SB_[1], SB_[2], SB_[3]]
            kvp = P4[:, 0:1024]
            tpB = bfv(P4[:, 1024:1536])
            tpK = bfv(P4[:, 1536:2048])
            vcnt = [0]

            def rsq(SS, m, n):
                tg = SS["tag"]
                T.op("act", lambda e: e.activation(out=SS["st2"][:, 0:m], in_=SS["st"][:, 0:m], func=AF.Sqrt, scale=1.0 / n, bias=EPS),
                     reads=["st_" + tg], writes=["st2_" + tg])
                T.op("dve", lambda e: e.reciprocal(out=SS["rs"][:, 0:m], in_=SS["st2"][:, 0:m]),
                     reads=["st2_" + tg], writes=["rs_" + tg])

            def norm_T(xt, xk, Aa, Bb, Ak, Bk):
                T.op("act", lambda e: e.activation(out=SX["junk"][:], in_=xt[:], func=AF.Square, accum_out=SX["st"][:, 0:1]),
                     reads=[xk], writes=["junk_x", "st_x"])
                rsq(SX, 1, float(D))
                T.op("dve", lambda e: e.tensor_scalar(out=xn[:], in0=xt[:], scalar1=SX["rs"][:, 0:1], scalar2=None, op0=ALU.mult),
                     reads=[xk, "rs_x"], writes=["xn"])
                for c in range(8):
                    T.op("pe", lambda e: e.transpose(out=tp[:, c, :], in_=xn[:, c * 128:(c + 1) * 128], identity=ident_bf[:]),
                         reads=["xn", "ident_bf"], writes=["S0"])
                T.op("act", lambda e: e.activation(out=hT[:], in_=tp[:, :, :], func=AF.Copy), reads=["S0"], writes=["hT"])

            def proj(ps, pk, c0, c1):
                for c in range(8):
                    T.op("pe", lambda e: e.matmul(ps[:, 0:c1 - c0], lhsT=hT[:, c, :], rhs=w_in_sb[:, c, c0:c1],
                                                  start=(c == 0), stop=False), reads=["hT", "w_in_sb"], writes=[pk])
                T.op("pe", lambda e: e.matmul(ps[:, 0:c1 - c0], lhsT=ones_bf[0:1, :], rhs=brow[0:1, c0:c1], start=False, stop=True),
                     reads=["ones_bf", "brow"], writes=[pk])

            def lowrank(ps_ap, pk, n, gfm, gk, wsb, wk, outw):
                width = n * 128
                T.op("act", lambda e: e.activation(out=SC["junk"][:, 0:width], in_=ps_ap, func=AF.Square, accum_out=SC["st"][:, 0:1]),
                     reads=[pk], writes=["junk_c", "st_c"])
                rsq(SC, 1, float(width))
                T.op("dve", lambda e: e.tensor_scalar(out=cs[:, 0:width], in0=ps_ap, scalar1=SC["rs"][:, 0:1], scalar2=None,
                                                      op0=ALU.mult), reads=[pk, "rs_c"], writes=["cs"])
                for c in range(n):
                    T.op("pe", lambda e: e.transpose(out=tpB[:, c, :], in_=cs[:, c * 128:(c + 1) * 128], identity=ident_bf[:]),
                         reads=["cs", "ident_bf"], writes=["tpB"])
                for c in range(n):
                    T.op("act", lambda e: e.activation(out=cT_sb[:, c, :], in_=tpB[:, c, :], func=AF.Copy,
                                                       scale=gfm[:, c:c + 1]), reads=["tpB", gk], writes=["cT_sb"])
                for h0 in range(0, outw, 512):
                    h1 = min(outw, h0 + 512)
                    for c in range(n):
                        T.op("pe", lambda e: e.matmul(kvp[:, h0:h1], lhsT=cT_sb[:, c, :], rhs=wsb[:, c, h0:h1],
                                                      start=(c == 0), stop=(c == n - 1)),
                             reads=["cT_sb", wk], writes=["kvp"])

            def rope(src, dst, cosb, sinb, ck, sk_, srck, dstk, nh, rt, rtk_):
                shp = [128, nh, 16]
                cb = cosb.unsqueeze(1).to_broadcast(shp)
                sbb = sinb.unsqueeze(1).to_broadcast(shp)
                x1 = src[:, :, 0:16]
                x2 = src[:, :, 16:32]
                r = [rt[:, 0:nh, i * 16:(i + 1) * 16] for i in range(4)]
                T.op("dve", lambda e: e.tensor_tensor(out=r[0], in0=x1, in1=cb, op=ALU.mult), reads=[srck, ck], writes=[rtk_ + "0"])
                T.op("dve", lambda e: e.tensor_tensor(out=r[1], in0=x2, in1=sbb, op=ALU.mult), reads=[srck, sk_], writes=[rtk_ + "1"])
                T.op("dve", lambda e: e.tensor_tensor(out=r[2], in0=x2, in1=cb, op=ALU.mult), reads=[srck, ck], writes=[rtk_ + "2"])
                T.op("dve", lambda e: e.tensor_tensor(out=r[3], in0=x1, in1=sbb, op=ALU.mult), reads=[srck, sk_], writes=[rtk_ + "3"])
                T.op("dve", lambda e: e.tensor_tensor(out=dst[:, :, 0:16], in0=r[0], in1=r[1], op=ALU.subtract),
                     reads=[rtk_ + "0", rtk_ + "1"], writes=[dstk])
                T.op("dve", lambda e: e.tensor_tensor(out=dst[:, :, 16:32], in0=r[2], in1=r[3], op=ALU.add),
                     reads=[rtk_ + "2", rtk_ + "3"], writes=[dstk])

            def head_sumsq(SS, ps_ap, pk, width, nh, dh, lo, hi):
                tg = SS["tag"]
                T.op("act", lambda e: e.activation(out=SS["junk"][:, 0:width], in_=ps_ap, func=AF.Square), reads=[pk], writes=["junk_" + tg])
                jv = SS["junk"][:, 0:width].rearrange("p (h d) -> p h d", d=dh)[:, :, lo:hi]
                T.op("dve", lambda e: e.tensor_reduce(out=SS["st"][:, 0:nh], in_=jv, axis=AX.X, op=ALU.add),
                     reads=["junk_" + tg], writes=["st_" + tg])

            def fox_qk(ps, pk, gbc, gk, dstT_main, col0):
                head_sumsq(SF, ps[:, :], pk, 512, 8, 64, 0, 64)
                rsq(SF, 8, 64.0)
                pv = ps[:, :].rearrange("p (h d) -> p h d", d=64)
                T.op("dve", lambda e: e.tensor_tensor(out=qgf[:], in0=pv, in1=gbc[:].unsqueeze(1).to_broadcast([128, 8, 64]),
                                                      op=ALU.mult), reads=[pk, gk], writes=["qgf"])
                T.op("dve", lambda e: e.tensor_tensor(out=Ft[:], in0=qgf[:], in1=SF["rs"][:, 0:8].unsqueeze(2).to_broadcast([128, 8, 64]),
                                                       op=ALU.mult), reads=["qgf", "rs_f"], writes=["Ft"])
                fv_ = Ft[:].rearrange("p (q two) d -> p q (two d)", two=2)
                for q in range(4):
                    T.op("pe", lambda e: e.transpose(out=tpB[:, 4 + q, :], in_=fv_[:, q, :], identity=ident_bf[:]),
                         reads=["Ft", "ident_bf"], writes=["tpB"])
                T.op("act", lambda e: e.activation(out=FTt[:], in_=tpB[:, 4:8, :], func=AF.Copy), reads=["tpB"], writes=["FTt"])
                dv = dstT_main.rearrange("(q two) d t -> (two d) q t", two=2)[:, :, col0:col0 + 128]
                T.dma("sp", dv, FTt[:], reads=["FTt"], key="st_FTt")

            def kv_front(ti, ui):
                s = ui % 2
                xt, xk = xa[s], f"xa{s}"
                norm_T(xt, xk, A1, B1, "A1", "B1")
                proj(pj[0], "S1", 384, 672)
                for c in range(8):
                    T.op("pe", lambda e: e.matmul(pj[0][0:8, 384:512], lhsT=w_in_sb[:, c, 2208:2216], rhs=hT[:, c, :],
                                                  start=(c == 0), stop=False), reads=["hT", "w_in_sb"], writes=["S1"])
                T.op("pe", lambda e: e.matmul(pj[0][0:8, 384:512], lhsT=brow[0:1, 2208:2216], rhs=ones_bf[0:1, :], start=False, stop=True),
                     reads=["ones_bf", "brow"], writes=["S1"])
                proj(pj[1], "S2", 1184, 1696)
                proj(pj[2], "S3", 1696, 2208)

            def kv_mid(ti):
                T.op("act", lambda e: e.activation(out=FLT[:, ti * 128:(ti + 1) * 128], in_=pj[0][0:8, 384:512], func=AF.Copy),
                     reads=["S1"], writes=["FLT"])
                T.op("act", lambda e: e.activation(out=junk_p[:], in_=pj[0][:, 256:288], func=AF.Square, accum_out=sm[:, 0:1]),
                     reads=["S1"], writes=["junk_p", "sm"])
                T.op("dve", lambda e: e.tensor_tensor(out=kpg[:], in0=pj[0][:, 256:288], in1=gk_bc[:, 64:96], op=ALU.mult),
                     reads=["S1", "gk_bc"], writes=["kpg"])
                lowrank(pj[0][:, 0:256], "S1", 2, gkva, "gkva", w_ukv_sb, "w_ukv_sb", 1024)
                rope(kpg[:].unsqueeze(1), kr[:].unsqueeze(1), cos_all[:, ti, :], sin_all[:, ti, :], "cos_all", "sin_all", "kpg", "kr", 1,
                     rtk, "rtk")
                vs = vcnt[0] % 2
                vcnt[0] += 1
                T.op("act", lambda e: e.activation(out=Vt[vs][:, :, 0:64], in_=pj[2][:, :].rearrange("p (h d) -> p h d", d=64),
                                                   func=AF.Copy), reads=["S3"], writes=[f"Vt{vs}"])
                T.dma("sp", V_fox[ti], Vt[vs][:], reads=[f"Vt{vs}"], key=f"st_Vt{vs}")
                fox_qk(pj[1], "S2", gfk_bc, "gfk_bc", KTF_main, ti * 128)

            def kv_tail(ti):
                head_sumsq(SK, kvp, "kvp", 1024, 8, 128, 0, 64)
                T.op("dve", lambda e: e.tensor_scalar(out=SK["st"][:, 0:8], in0=SK["st"][:, 0:8], scalar1=sm[:, 0:1], scalar2=None, op0=ALU.add),
                     reads=["st_k", "sm"], writes=["st_k"])
                rsq(SK, 8, 96.0)
                kvv = kvp.rearrange("p (h d) -> p h d", d=128)
                vs = vcnt[0] % 2
                vcnt[0] += 1
                T.op("act", lambda e: e.activation(out=Vt[vs][:, :, 0:64], in_=kvv[:, :, 64:128], func=AF.Copy),
                     reads=["kvp"], writes=[f"Vt{vs}"])
                T.dma("sp", V_mla[ti], Vt[vs][:], reads=[f"Vt{vs}"], key=f"st_Vt{vs}")
                T.op("dve", lambda e: e.tensor_tensor(out=qg[:, :, 0:64], in0=kvv[:, :, 0:64],
                                                      in1=gk_bc[:, 0:64].unsqueeze(1).to_broadcast([128, 8, 64]), op=ALU.mult),
                     reads=["kvp", "gk_bc"], writes=["qg"])
                T.op("dve", lambda e: e.tensor_tensor(out=Kt[:, :, 0:64], in0=qg[:, :, 0:64],
                                                      in1=SK["rs"][:, 0:8].unsqueeze(2).to_broadcast([128, 8, 64]), op=ALU.mult),
                     reads=["qg", "rs_k"], writes=["Kt"])
                T.op("dve", lambda e: e.tensor_tensor(out=Kt[:, :, 64:96], in0=kr[:].unsqueeze(1).to_broadcast([128, 8, 32]),
                                                       in1=SK["rs"][:, 0:8].unsqueeze(2).to_broadcast([128, 8, 32]), op=ALU.mult),
                     reads=["kr", "rs_k"], writes=["Kt"])
                for h in range(8):
                    T.op("pe", lambda e: e.transpose(out=tpK[0:96, h, :], in_=Kt[:, h, :], identity=ident_bf[:]),
                         reads=["Kt", "ident_bf"], writes=["tpK"])
                T.op("act", lambda e: e.activation(out=KTt[0:96, :, :], in_=tpK[0:96, :, :], func=AF.Copy), reads=["tpK"], writes=["KTt"])
                T.dma("sp", KT_mla[:, :, ti * 128:(ti + 1) * 128].rearrange("h d t -> d h t"), KTt[0:96, :, :], reads=["KTt"], key="st_KTt")

            def q_front(k, ui):
                s = ui % 2
                xt, xk = xa[s], f"xa{s}"
                norm_T(xt, xk, A1, B1, "A1", "B1")
                proj(pj[0], "S1", 0, 384)
                proj(pj[1], "S2", 672, 1184)

            def q_mid(k):
                lowrank(pj[0][:, 0:384], "S1", 3, gqa, "gqa", w_uq_sb, "w_uq_sb", 768)
                fox_qk(pj[1], "S2", gfq_bc, "gfq_bc", QTF_main, k * 128)

            def q_tail(k):
                qp_ = kvp[:, 0:768]
                head_sumsq(SK, qp_, "kvp", 768, 8, 96, 0, 96)
                rsq(SK, 8, 96.0)
                T.op("dve", lambda e: e.tensor_tensor(out=qg[:], in0=qp_.rearrange("p (h d) -> p h d", d=96),
                                                      in1=gq_bc[:].unsqueeze(1).to_broadcast([128, 8, 96]), op=ALU.mult),
                     reads=["kvp", "gq_bc"], writes=["qg"])
                rope(qg[:, :, 64:96], qg[:, :, 64:96], cos_own[:, k, :], sin_own[:, k, :], "cos_own", "sin_own", "qg", "qg", 8, rtq, "rtq")
                T.op("dve", lambda e: e.tensor_tensor(out=Kt[:], in0=qg[:], in1=SK["rs"][:, 0:8].unsqueeze(2).to_broadcast([128, 8, 96]),
                                                      op=ALU.mult), reads=["qg", "rs_k"], writes=["Kt"])
                for h in range(8):
                    T.op("pe", lambda e: e.transpose(out=tpK[0:96, h, :], in_=Kt[:, h, :], identity=ident_bf[:]),
                         reads=["Kt", "ident_bf"], writes=["tpK"])
                T.op("act", lambda e: e.activation(out=KTt[0:96, :, :], in_=tpK[0:96, :, :], func=AF.Copy), reads=["tpK"], writes=["KTt"])
                T.dma("sp", QT_mla[:, :, k * 128:(k + 1) * 128].rearrange("h d t -> d h t"), KTt[0:96, :, :], reads=["KTt"], key="st_KTt")

            units = []
            for k in range(NO):
                units.append((kv_front, kv_mid, kv_tail, 2 * k))
                units.append((kv_front, kv_mid, kv_tail, 2 * k + 1))
                units.append((q_front, q_mid, q_tail, k))
            def load_x(ui):
                fr_, _, _, arg_ = units[ui]
                src = x_all if fr_ is kv_front else x_own
                T.dma("sp", xa[ui % 2][:], src[arg_ * 128:(arg_ + 1) * 128, :], writes=[f"xa{ui % 2}"], key=f"ld_xa{ui % 2}")

            prev_u = None
            load_x(0)
            for ui, (fr, md, tl, arg) in enumerate(units):
                if ui % 3 == 0:
                    issue_cast()
                if ui + 1 < len(units):
                    load_x(ui + 1)
                fr(arg, ui)
                if prev_u is not None:
                    prev_u[0](prev_u[1])
                md(arg)
                prev_u = (tl, arg)
            prev_u[0](prev_u[1])
            while cast_jobs:
                issue_cast()

            T.drain()

        with ExitStack() as pf_:
            bfc = sb(pf_, [8, 1], F32, "bfc")
            nbf = sb(pf_, [8, 1], F32, "nbf")
            w1 = sb(pf_, [8, S], F32, "w1")
            fo = sb(pf_, [8, SO], F32, "fo")
            aug = sb(pf_, [8, 3, S], BF16, "aug")
            onesb = sb(pf_, [8, 3, 2048], BF16, "onesb")
            ld(bfc[:], bf_col[:, :], "bfc")
            T.op("dve", lambda e: e.tensor_scalar(out=nbf[:], in0=bfc[:], scalar1=-1.0, scalar2=None, op0=ALU.mult),
                 reads=["bfc"], writes=["nbf"])
            T.op("dve", lambda e: e.memset(onesb[:], 1.0), writes=["onesb"])
            for q in range(4):
                T.dma("sp", KTF_aug[:, 0:3, q * 2048:(q + 1) * 2048], onesb[:], reads=["onesb"], key="st_onesb")
            for q in range(2):
                T.dma("sp", QTF_aug[:, 3:6, q * 2048:(q + 1) * 2048], onesb[:], reads=["onesb"], key="st_onesb")
            T.op("act", lambda e: e.activation(out=w1[:], in_=FLT[:], func=AF.Exp, bias=nbf[:, 0:1], scale=-1.0),
                 reads=["FLT", "nbf"], writes=["w1"])
            T.op("act", lambda e: e.activation(out=w1[:], in_=w1[:], func=AF.Ln, bias=1.0, scale=1.0), reads=["w1"], writes=["w1"])
            T.op("dve", lambda e: e.tensor_scalar(out=w1[:], in0=w1[:], scalar1=-1.0, scalar2=None, op0=ALU.mult),
                 reads=["w1"], writes=["w1"])
            T.op("dve", lambda e: e.tensor_tensor_scan(out=FLT[:], data0=ones_f[0:8, 0:1].to_broadcast([8, S]), data1=w1[:],
                                                       initial=0.0, op0=ALU.mult, op1=ALU.add), reads=["w1", "ones_f"], writes=["FLT"])

            def split3(src, srck, n, sign, tmpa, tmpk):
                cur, curk = src, srck
                for i in range(3):
                    sg = sign if i == 0 else 1.0
                    T.op("dve", lambda e: e.tensor_scalar(out=aug[:, i, 0:n], in0=cur, scalar1=sg, scalar2=None, op0=ALU.mult),
                         reads=[curk], writes=["aug"])
                    if i < 2:
                        T.op("dve", lambda e: e.scalar_tensor_tensor(out=tmpa, in0=cur, scalar=sg, in1=aug[:, i, 0:n],
                                                                     op0=ALU.mult, op1=ALU.subtract), reads=[curk, "aug"], writes=[tmpk])
                        cur, curk = tmpa, tmpk

            split3(FLT[:], "FLT", S, -1.0, w1[:], "w1")
            T.dma("sp", KTF_aug[:, 3:6, :], aug[:], reads=["aug"], key="st_aug")
            fv4 = FLT[:].rearrange("p (k two r) -> p k two r", two=2, r=128)
            fo3 = fo[:].rearrange("p (k r) -> p k r", r=128)
            T.op("dve", lambda e: e.tensor_tensor(out=fo3, in0=fv4[:, :, 1, :], in1=fv4[:, :, 0, :], op=ALU.subtract),
                 reads=["FLT"], writes=["fo"])
            T.op("dve", lambda e: e.scalar_tensor_tensor(out=fo3, in0=fo3, scalar=parc[0:8, 0:1], in1=fv4[:, :, 0, :],
                                                         op0=ALU.mult, op1=ALU.add), reads=["fo", "parc", "FLT"], writes=["fo"])
            split3(fo[:], "fo", SO, 1.0, w1[:, 0:SO], "w1")
            T.dma("sp", QTF_aug[:, 0:3, :], aug[:, :, 0:SO], reads=["aug"], key="st_aug")
            T.drain()
        pfl.close()

        if STOP_AFTER == "B":
            T.drain()
            return nc

        with ExitStack() as pc:
            KTs = [sb(pc, [96, S], BF16, f"KTs{i}") for i in range(2)]
            QTs = [sb(pc, [96, SO], BF16, f"QTs{i}") for i in range(2)]
            Vs = [sb(pc, [128, NT, 65], BF16, f"Vs{i}") for i in range(2)]
            pT = [sb(pc, [128, 2, 512], BF16, f"pT{i}") for i in range(3)]
            osb = [sb(pc, [64, 512], F32, f"osb{i}") for i in range(2)]
            rrow = sb(pc, [1, 512], F32, "rrow")
            rr2 = sb(pc, [33, 512], BF16, "rr2")
            ones33 = sb(pc, [33, 64], BF16, "ones33")
            T.op("dve", lambda e: e.memset(rr2[:], 0.0), writes=["rr2"])
            T.op("dve", lambda e: e.memset(ones33[:], 1.0), writes=["ones33"])
            oT = [sb(pc, [64, 512], BF16, f"oT{i}") for i in range(2)]
            s2 = [Q4[:, 0:1024].rearrange("p (b c) -> p b c", c=512), Q4[:, 1024:2048].rearrange("p (b c) -> p b c", c=512)]
            ops_ = [P4[:, 0:512], P4[:, 512:1024]]
            bcp = P4[:, 1024:1536]

            def load_head(hh):
                s = hh % 2
                if hh < 8:
                    T.dma("sp", KTs[s][0:96, :], KT_mla[hh], writes=[f"KTs{s}"], key=f"ld_KTs{s}")
                    T.dma("sp", QTs[s][0:96, :], QT_mla[hh], writes=[f"QTs{s}"], key=f"ld_QTs{s}")
                    T.dma("sp", Vs[s][:], V_mla[:, :, hh, :].rearrange("t p e -> p t e"), writes=[f"Vs{s}"], key=f"ld_Vs{s}")
                else:
                    h = hh - 8
                    T.dma("sp", KTs[s][0:64, :], KTF_main[h], writes=[f"KTs{s}"], key=f"ld_KTs{s}")
                    T.dma("sp", KTs[s][64:70, :], KTF_aug[h], writes=[f"KTs{s}"], key=f"ld_KTs{s}")
                    T.dma("sp", QTs[s][0:64, :], QTF_main[h], writes=[f"QTs{s}"], key=f"ld_QTs{s}")
                    T.dma("sp", QTs[s][64:70, :], QTF_aug[h], writes=[f"QTs{s}"], key=f"ld_QTs{s}")
                    T.dma("sp", Vs[s][:], V_fox[:, :, h, :].rearrange("t p e -> p t e"), writes=[f"Vs{s}"], key=f"ld_Vs{s}")

            step = [0]
            unit = [0]
            pending_norm = [None]

            def emit_S(hh, g, m):
                s = hh % 2
                dk = 96 if hh < 8 else 70
                n = step[0]
                sp_ = s2[n % 2]
                spk = f"Q{n % 2}"
                jj0 = 2 * m - 8 * g
                diag = jj0 >= 0
                kq = jj0 // 2 if diag else 0
                c0 = kq * 128
                for b_ in range(2):
                    j = 2 * m + b_
                    T.op("pe", lambda e: e.matmul(sp_[:, b_, c0:512], lhsT=KTs[s][0:dk, j * 128:(j + 1) * 128],
                                                  rhs=QTs[s][0:dk, g * 512 + c0:(g + 1) * 512], start=True, stop=not diag),
                         reads=[f"KTs{s}", f"QTs{s}"], writes=[spk])
                    if diag:
                        mk, mkk = (maskA, "maskA") if b_ == 0 else (maskB, "maskB")
                        T.op("pe", lambda e: e.matmul(sp_[:, b_, kq * 128:(kq + 1) * 128], lhsT=ident_bf[:], rhs=mk[:],
                                                      start=False, stop=True), reads=["ident_bf", mkk], writes=[spk])
                return (n, c0)

            def emit_exp_pv(hh, g, m, n, c0, first, last):
                s = hh % 2
                sp_ = s2[n % 2]
                pt = pT[n % 3]
                u = unit[0]
                op_ = ops_[u % 2]
                T.op("act", lambda e: e.activation(out=pt[:, :, c0:512], in_=sp_[:, :, c0:512], func=AF.Exp),
                     reads=[f"Q{n % 2}"], writes=[f"pT{n % 3}"])
                for b_ in range(2):
                    j = 2 * m + b_
                    T.op("pe", lambda e: e.matmul(op_[0:65, c0:512], lhsT=Vs[s][:, j, :], rhs=pt[:, b_, c0:512],
                                                  start=(first and b_ == 0), stop=(last and b_ == 1)),
                         reads=[f"Vs{s}", f"pT{n % 3}"], writes=[f"ops{u % 2}"])

            def emit_norm_a(hh, g, u):
                ob = osb[u % 2]
                T.op("act", lambda e: e.activation(out=ob[:], in_=ops_[u % 2][0:64, :], func=AF.Copy),
                     reads=[f"ops{u % 2}"], writes=[f"osb{u % 2}"])
                T.op("dve", lambda e: e.reciprocal(out=rrow[0:1, :], in_=ops_[u % 2][64:65, :]), reads=[f"ops{u % 2}"], writes=["rrow"])
                T.op("dve", lambda e: e.tensor_copy(out=rr2[0:1, :], in_=rrow[0:1, :]), reads=["rrow"], writes=["rr2"])
                T.op("dve", lambda e: e.tensor_tensor(out=rr2[32:33, :], in0=rrow[0:1, :], in1=rr2[0:1, :], op=ALU.subtract),
                     reads=["rrow", "rr2"], writes=["rr2"])

            def emit_norm_b(hh, g, u):
                ob = osb[u % 2]
                T.op("pe", lambda e: e.matmul(bcp[0:64, :], lhsT=ones33[:, :], rhs=rr2[:, :], start=True, stop=True),
                     reads=["ones33", "rr2"], writes=["P4b"])
                T.op("dve", lambda e: e.tensor_tensor(out=oT[u % 2][:], in0=ob[0:64, :], in1=bcp[0:64, :], op=ALU.mult),
                     reads=[f"osb{u % 2}", "P4b"], writes=[f"oT{u % 2}"])
                T.dma("sp", OT[hh * 64:(hh + 1) * 64, g * 512:(g + 1) * 512], oT[u % 2][:], reads=[f"oT{u % 2}"], key=f"st_oT{u % 2}")

            load_head(0)
            for hh in range(16):
                if hh + 1 < 16:
                    load_head(hh + 1)
                for g in range(8):
                    npair = 4 * g + 4
                    prev = None
                    for m in range(npair):
                        cur = emit_S(hh, g, m)
                        step[0] += 1
                        if prev is not None:
                            emit_exp_pv(hh, g, m - 1, prev[0], prev[1], m - 1 == 0, False)
                        if m == 1 and pending_norm[0] is not None:
                            emit_norm_b(*pending_norm[0])
                            pending_norm[0] = None
                        prev = cur
                    emit_exp_pv(hh, g, npair - 1, prev[0], prev[1], False, True)
                    emit_norm_a(hh, g, unit[0])
                    pending_norm[0] = (hh, g, unit[0])
                    unit[0] += 1
            emit_norm_b(*pending_norm[0])
            T.drain()

        if STOP_AFTER == "C":
            return nc

        with ExitStack() as pd:
            w_o_sb = sb(pd, [128, 8, D], BF16, "w_o_sb")
            w_pq_sb = sb(pd, [128, 8, 2048], BF16, "w_pq_sb")
            skT_sb = sb(pd, [128, 16, 128], F32, "skT_sb")
            T.dma("pool", w_o_sb[:], w_o.rearrange("(c p) n -> p c n", p=128), writes=["w_o_sb"], key="ld_w_o")
            for c in range(8):
                T.dma("pool", w_pq_sb[:, c, :], w_pq[c * 128:(c + 1) * 128, :], writes=["w_pq_sb"], key="ld_w_pq",
                      max_dma_last_dim=4096)
            ld(skT_sb[:], skT.rearrange("p (a n) -> p a n", n=128), "skT_sb")
            with ExitStack() as pg:
                gate1 = sb(pg, [128, D], F32, "gate1d")
                ld(gate1[:], G1S[:, :], "gate1d")
                T.op("dve", lambda e: e.tensor_tensor(out=w_o_sb[:], in0=w_o_sb[:], in1=gate1[:].unsqueeze(1).to_broadcast([128, 8, D]),
                                                      op=ALU.mult), reads=["w_o_sb", "gate1d"], writes=["w_o_sb"])
                T.drain()
            oTs = [sb(pd, [128, 8, 128], BF16, f"oTs{i}") for i in range(2)]
            xo = [sb(pd, [128, D], F32, f"xo{i}") for i in range(2)]
            x1 = sb(pd, [128, D], F32, "x1")
            junk = sb(pd, [128, D], F32, "junkd")
            xn = sb(pd, [128, D], BF16, "xnd")
            hT = sb(pd, [128, 8, 128], BF16, "hTd")
            st = sb(pd, [128, 8], F32, "std")
            st2 = sb(pd, [128, 8], F32, "st2d")
            rs = sb(pd, [128, 8], F32, "rsd")
            qpT = sb(pd, [128, 16, 128], F32, "qpT")
            scs2 = [sb(pd, [128, 16, 128], F32, f"scs{i}") for i in range(2)]
            scw = sb(pd, [128, 16, 128], F32, "scw")
            top = sb(pd, [128, 16, 16], F32, "top")
            tix = sb(pd, [128, 16, 16], U32, "tix")
            tixf = sb(pd, [128, 16, 16], F32, "tixf")
            cand = sb(pd, [128, 8, 256], F32, "cand")
            candw = sb(pd, [128, 8, 256], F32, "candw")
            ts = sb(pd, [128, 8, 16], F32, "ts")
            tc = sb(pd, [128, 8, 16], U32, "tc")
            tca = sb(pd, [128, 8, 16], U32, "tca")
            tcb = sb(pd, [128, 8, 16], U32, "tcb")
            taf = sb(pd, [128, 8, 16], F32, "taf")
            tbf = sb(pd, [128, 8, 16], F32, "tbf")
            ohs = [[sb(pd, [128, 8, 16, 16], F32, f"oh{i}{j}") for j in range(2)] for i in range(2)]
            ees = [sb(pd, [128, 3, 128], F32, f"ee{i}") for i in range(2)]
            eT = sb(pd, [128, 3, 128], F32, "eT")
            gsum = sb(pd, [128, 8], F32, "gsum")
            tp = bfv(SB_[0][:, :])
            scp = P4

            def norm_T2(xt, xk):
                T.op("act", lambda e: e.activation(out=junk[:], in_=xt[:], func=AF.Square, accum_out=st[:, 0:1]),
                     reads=[xk], writes=["junkd", "std"])
                T.op("pool", lambda e: e.tensor_scalar(out=st2[:, 0:1], in0=st[:, 0:1], scalar1=1.0 / D, scalar2=EPS,
                                                       op0=ALU.mult, op1=ALU.add), reads=["std"], writes=["st2d"])
                T.op("pool", lambda e: e.tensor_tensor(out=rs[:, 0:1], in0=st2[:, 0:1], in1=mhalf[:, 0:1], op=ALU.pow),
                     reads=["st2d", "mhalf"], writes=["rsd"])
                T.op("act", lambda e: e.activation(out=xn[:], in_=xt[:], func=AF.Copy, scale=rs[:, 0:1]),
                     reads=[xk, "rsd"], writes=["xnd"])
                for c in range(8):
                    T.op("pe", lambda e, c=c: e.transpose(out=tp[:, c, :], in_=xn[:, c * 128:(c + 1) * 128], identity=ident_bf[:]),
                         reads=["xnd", "ident_bf"], writes=["S0"])
                for c in range(8):
                    T.op("act", lambda e, c=c: e.activation(out=hT[:, c, :], in_=tp[:, c, :], func=AF.Identity,
                                                           bias=B2[:, c:c + 1], scale=A2[:, c:c + 1]),
                         reads=["S0", "A2", "B2"], writes=["hTd"])

            def top16(src3, srck, work3, workk, nseg, width, vals, valk, idx, idxk):
                for s_ in range(nseg):
                    T.op("dve", lambda e, s_=s_: e.max(out=vals[:, s_, 0:8], in_=src3[:, s_, :]), reads=[srck], writes=[valk])
                    T.op("dve", lambda e, s_=s_: e.match_replace(out=work3[:, s_, :], in_to_replace=vals[:, s_, 0:8],
                                                                in_values=src3[:, s_, :], imm_value=-1e30),
                         reads=[srck, valk], writes=[workk])
                    T.op("dve", lambda e, s_=s_: e.max(out=vals[:, s_, 8:16], in_=work3[:, s_, :]), reads=[workk], writes=[valk])
                    T.op("dve", lambda e, s_=s_: e.max_index(out=idx[:, s_, 0:8], in_max=vals[:, s_, 0:8], in_values=src3[:, s_, :]),
                         reads=[srck, valk], writes=[idxk])
                    T.op("dve", lambda e, s_=s_: e.max_index(out=idx[:, s_, 8:16], in_max=vals[:, s_, 8:16], in_values=src3[:, s_, :]),
                         reads=[srck, valk], writes=[idxk])

            def d_load(k):
                s = k % 2
                T.dma("sp", oTs[s][:], OT[:, k * 128:(k + 1) * 128].rearrange("(c p) t -> p c t", p=128), writes=[f"oTs{s}"],
                      key=f"ld_oTs{s}")
                T.dma("sp", xo[s][:], x_own[k * 128:(k + 1) * 128, :], writes=[f"xo{s}"], key=f"ld_xo{s}")

            def d_front(k):
                s = k % 2
                for half in range(2):
                    for c in range(8):
                        T.op("pe", lambda e, c=c, half=half: e.matmul(SB_[1 + half][:, :], lhsT=oTs[s][:, c, :],
                                                                     rhs=w_o_sb[:, c, half * 512:(half + 1) * 512],
                                                                     start=(c == 0), stop=(c == 7)),
                             reads=[f"oTs{s}", "w_o_sb"], writes=[f"S{1 + half}"])
                for half in range(2):
                    sl = slice(half * 512, (half + 1) * 512)
                    T.op("act", lambda e, half=half, sl=sl: e.activation(out=x1[:, sl], in_=SB_[1 + half][:, :], func=AF.Copy),
                         reads=[f"S{1 + half}"], writes=["x1"])
                T.op("pool", lambda e: e.tensor_tensor(out=x1[:], in0=x1[:], in1=xo[s][:], op=ALU.add),
                     reads=["x1", f"xo{s}"], writes=["x1"])
                T.dma("sp", X1[k * 128:(k + 1) * 128, :], x1[:], reads=["x1"], key="st_x1")
                norm_T2(x1, "x1")
                T.dma("sp", H2T[:, :, k * 128:(k + 1) * 128], hT[:], reads=["hTd"], key="st_hTd")
                for q4 in range(4):
                    bank = SB_[1 + (q4 % 2)]
                    bk = f"S{1 + (q4 % 2)}"
                    for a in range(4):
                        hp = q4 * 4 + a
                        for c in range(8):
                            T.op("pe", lambda e, c=c, hp=hp, a=a, bank=bank: e.matmul(
                                bank[:, a * 128:(a + 1) * 128], lhsT=w_pq_sb[:, c, hp * 128:(hp + 1) * 128], rhs=hT[:, c, :],
                                start=(c == 0), stop=(c == 7)), reads=["w_pq_sb", "hTd"], writes=[bk])
                    T.op("act", lambda e, q4=q4, bank=bank: e.activation(
                        out=qpT[:, q4 * 4:(q4 + 1) * 4, :], in_=bank[:, :].rearrange("p (a t) -> p a t", t=128), func=AF.Copy),
                        reads=[bk], writes=["qpT"])
                for hp in range(16):
                    T.op("pe", lambda e, hp=hp: e.matmul(scp[:, hp * 128:(hp + 1) * 128], lhsT=qpT[:, hp, :], rhs=skT_sb[:, hp, :],
                                                         start=True, stop=True), reads=["qpT", "skT_sb"], writes=["P4"])
                T.op("act", lambda e: e.activation(out=scs2[s][:], in_=scp[:, :].rearrange("p (a n) -> p a n", n=128), func=AF.Copy),
                     reads=["P4"], writes=[f"scs{s}"])

            def d_tail(k):
                s = k % 2
                scs = scs2[s]
                top16(scs, f"scs{s}", scw, "scw", 16, 128, top, "top", tix, "tix")
                T.op("dve", lambda e: e.tensor_copy(out=tixf[:], in_=tix[:]), reads=["tix"], writes=["tixf"])
                tv = top[:].rearrange("p (h two) a -> p h two a", two=2)
                s1t = tv[:, :, 0, :]
                s2t = tv[:, :, 1, :]
                c4 = cand[:].rearrange("p h (a b) -> p h a b", b=16)
                T.op("dve", lambda e: e.tensor_tensor(out=c4, in0=s1t.unsqueeze(3).to_broadcast([128, 8, 16, 16]),
                                                      in1=s2t.unsqueeze(2).to_broadcast([128, 8, 16, 16]), op=ALU.add),
                     reads=["top"], writes=["cand"])
                top16(cand, "cand", candw, "candw", 8, 256, ts, "ts", tc, "tc")
                T.op("dve", lambda e: e.tensor_single_scalar(out=tca[:], in_=tc[:], scalar=4, op=ALU.arith_shift_right),
                     reads=["tc"], writes=["tca"])
                T.op("dve", lambda e: e.tensor_single_scalar(out=tcb[:], in_=tc[:], scalar=15, op=ALU.bitwise_and),
                     reads=["tc"], writes=["tcb"])
                T.op("dve", lambda e: e.tensor_copy(out=taf[:], in_=tca[:]), reads=["tca"], writes=["taf"])
                T.op("dve", lambda e: e.tensor_copy(out=tbf[:], in_=tcb[:]), reads=["tcb"], writes=["tbf"])
                iv = tixf[:].rearrange("p (h two) a -> p h two a", two=2)
                io16 = iota[:, 0:16].unsqueeze(1).unsqueeze(1).to_broadcast([128, 8, 16, 16])
                ee = ees[s]
                eek = f"ee{s}"
                for (sel, selk, which) in ((taf, "taf", 0), (tbf, "tbf", 1)):
                    oh = ohs[s][which]
                    ohk = f"oh{s}{which}"
                    T.op("dve", lambda e: e.tensor_tensor(out=oh[:], in0=sel[:].unsqueeze(3).to_broadcast([128, 8, 16, 16]),
                                                          in1=io16, op=ALU.is_equal), reads=[selk, "iota"], writes=[ohk])
                    T.op("dve", lambda e: e.tensor_tensor(
                        out=oh[:], in0=oh[:], in1=iv[:, :, which, :].unsqueeze(2).to_broadcast([128, 8, 16, 16]), op=ALU.mult),
                        reads=[ohk, "tixf"], writes=[ohk])
                g3 = ee[:, 2, :].rearrange("p (h k) -> p h k", k=16)
                T.op("dve", lambda e: e.tensor_tensor(out=g3, in0=ts[:], in1=ts[:, :, 0:1].to_broadcast([128, 8, 16]), op=ALU.subtract),
                     reads=["ts"], writes=[eek])
                T.op("act", lambda e: e.activation(out=g3, in_=g3, func=AF.Exp), reads=[eek], writes=[eek])
                T.op("dve", lambda e: e.tensor_reduce(out=gsum[:], in_=g3, axis=AX.X, op=ALU.add), reads=[eek], writes=["gsum"])
                T.op("dve", lambda e: e.reciprocal(out=gsum[:], in_=gsum[:]), reads=["gsum"], writes=["gsum"])
                T.op("dve", lambda e: e.tensor_tensor(out=g3, in0=g3, in1=gsum[:].unsqueeze(2).to_broadcast([128, 8, 16]), op=ALU.mult),
                     reads=[eek, "gsum"], writes=[eek])

            def d_tail2(k):
                s = k % 2
                ee = ees[s]
                eek = f"ee{s}"
                for which in range(2):
                    T.op("dve", lambda e: e.tensor_reduce(out=ee[:, which, :].rearrange("p (h k) -> p h k", k=16), in_=ohs[s][which][:],
                                                          axis=AX.X, op=ALU.add), reads=[f"oh{s}{which}"], writes=[eek])
                for a in range(3):
                    T.op("pe", lambda e: e.transpose(out=SB_[3][:, a * 128:(a + 1) * 128], in_=ee[:, a, :], identity=ident_f[:]),
                         reads=[eek, "ident_f"], writes=["S3"])
                T.op("act", lambda e: e.activation(out=eT[:], in_=SB_[3][:, 0:384].rearrange("p (a t) -> p a t", t=128), func=AF.Copy),
                     reads=["S3"], writes=["eT"])
                T.dma("sp", ET[:, :, k * 128:(k + 1) * 128].rearrange("a p t -> p a t"), eT[:], reads=["eT"], key="st_eT")

            d_load(0)
            for k in range(NO):
                if k + 1 < NO:
                    d_load(k + 1)
                d_front(k)
                if k > 0:
                    d_tail(k - 1)
                if k > 1:
                    d_tail2(k - 2)
            d_tail(NO - 1)
            d_tail2(NO - 2)
            d_tail2(NO - 1)
            T.drain()

        if STOP_AFTER == "D":
            return nc

        with ExitStack() as pe_:
            TG = 256
            NG = SO // TG
            CH = 4
            NCH = TG // CH
            SBK = 4
            NSB = 128 // SBK
            Gs = [sb(pe_, [128, TG, 128], BF16, f"G{i}") for i in range(2)]
            NSL = 2
            Us = [sb(pe_, [128, SBK, 1024], BF16, f"Us{i}") for i in range(NSL)]
            Vs2 = [sb(pe_, [128, SBK, 1024], BF16, f"Vs2{i}") for i in range(NSL)]
            h2gs = [sb(pe_, [128, 8, TG], BF16, f"h2g{i}") for i in range(2)]
            etgs = [sb(pe_, [128, 3, TG], F32, f"etg{i}") for i in range(2)]
            x1g = sb(pe_, [128, D], F32, "x1g")
            og = sb(pe_, [128, D], F32, "og")
            E1 = [sb(pe_, [128, CH, 128], BF16, f"E1{i}") for i in range(2)]
            E2 = [sb(pe_, [128, CH, 128], BF16, f"E2{i}") for i in range(2)]
            ga = [sb(pe_, [128, TG], BF16, f"ga{i}") for i in range(4)]
            wT = [sb(pe_, [128, TG], BF16, f"wT{i}") for i in range(4)]
            yps = P4
            aps = [SB_[0], SB_[1], SB_[2]]
            gpb = SB_[3]
            Uv = Ubf.rearrange("(i p) (c j) -> p i (c j)", p=128, j=128)
            Vv = Vbf.rearrange("(i j) d -> j i d", j=128)
            io3 = iota[:].unsqueeze(1).to_broadcast([128, CH, 128])
            cctr = [0]

            TOT = NG * NSB

            def load_u(gsb):
                if gsb >= TOT:
                    return
                s, sbi = gsb % NSL, gsb % NSB
                T.dma("sp", Us[s][:], Uv[:, sbi * SBK:(sbi + 1) * SBK, :], writes=[f"Us{s}"], key=f"ld_Us{s}")

            def load_v(gsb):
                if gsb >= TOT:
                    return
                s, sbi = gsb % NSL, gsb % NSB
                T.dma("act", Vs2[s][:], Vv[:, sbi * SBK:(sbi + 1) * SBK, :], writes=[f"Vs2{s}"], key=f"ld_Vs2{s}")

            def load_group(gi):
                t0 = gi * TG
                T.dma("sp", h2gs[gi % 2][:], H2T[:, :, t0:t0 + TG], writes=[f"h2g{gi % 2}"], key=f"ld_h2g{gi % 2}")
                T.dma("sp", etgs[gi % 2][:], ET[:, :, t0:t0 + TG].rearrange("a p t -> p a t"), writes=[f"etg{gi % 2}"],
                      key=f"ld_etg{gi % 2}")

            def gb_prep(gi, ci):
                etg = etgs[gi % 2]
                ek = f"etg{gi % 2}"
                cs_ = ci % 2
                tt = slice(ci * CH, (ci + 1) * CH)
                e1b = etg[:, 0, tt].unsqueeze(2).to_broadcast([128, CH, 128])
                e2b = etg[:, 1, tt].unsqueeze(2).to_broadcast([128, CH, 128])
                gb = etg[:, 2, tt].unsqueeze(2).to_broadcast([128, CH, 128])
                T.op("dve", lambda e: e.tensor_tensor(out=E1[cs_][:], in0=io3, in1=e1b, op=ALU.is_equal),
                     reads=["iota", ek], writes=[f"E1{cs_}"])
                T.op("pool", lambda e: e.tensor_tensor(out=E1[cs_][:], in0=E1[cs_][:], in1=gb, op=ALU.mult),
                     reads=[f"E1{cs_}", ek], writes=[f"E1{cs_}"])
                T.op("dve", lambda e: e.tensor_tensor(out=E2[cs_][:], in0=io3, in1=e2b, op=ALU.is_equal),
                     reads=["iota", ek], writes=[f"E2{cs_}"])

            def gb_mm(gi, ci):
                G = Gs[gi % 2]
                Gk = f"G{gi % 2}"
                cs_ = ci % 2
                for a in range(4):
                    T.op("pe", lambda e: e.matmul(gpb[:, a * 128:(a + 1) * 128], lhsT=E2[cs_][:, a, :],
                                                  rhs=E1[cs_][:, a, :], start=True, stop=True),
                         reads=[f"E1{cs_}", f"E2{cs_}"], writes=["S3"])
                tb = ci * CH
                T.op("act", lambda e: e.activation(out=G[:, tb:tb + 4, :], in_=gpb[:, :].rearrange("p (a i) -> p a i", i=128),
                                                   func=AF.Copy), reads=["S3"], writes=[Gk])

            LOOK = 2

            def emit_A(gi, n):
                sbi, il = divmod(n, SBK)
                s = (gi * NSB + sbi) % NSL
                ap_ = aps[n % 3]
                apk = f"S{n % 3}"
                h2g = h2gs[gi % 2]
                for c in range(8):
                    T.op("pe", lambda e: e.matmul(ap_[:, 0:TG], lhsT=Us[s][:, il, c * 128:(c + 1) * 128],
                                                  rhs=h2g[:, c, :], start=(c == 0), stop=(c == 7)),
                         reads=[f"Us{s}", f"h2g{gi % 2}"], writes=[apk])
                T.op("act", lambda e: e.activation(out=ga[n % 4][:], in_=ap_[:, 0:TG], func=AF.Gelu_apprx_tanh),
                     reads=[apk], writes=[f"ga{n % 4}"])
                weng = "dve" if n % 2 == 0 else "pool"
                T.op(weng, lambda e: e.tensor_tensor(out=wT[n % 4][:], in0=ga[n % 4][:], in1=Gs[gi % 2][:, :, n], op=ALU.mult),
                     reads=[f"ga{n % 4}", f"G{gi % 2}"], writes=[f"wT{n % 4}"])

            def emit_Y(gi, n):
                sbi, il = divmod(n, SBK)
                s = (gi * NSB + sbi) % NSL
                for a in range(2):
                    for half in range(2):
                        T.op("pe", lambda e: e.matmul(
                            yps[:, (a * 2 + half) * 512:(a * 2 + half + 1) * 512], lhsT=wT[n % 4][:, a * 128:(a + 1) * 128],
                            rhs=Vs2[s][:, il, half * 512:(half + 1) * 512], start=(n == 0), stop=(n == 127)),
                            reads=[f"wT{n % 4}", f"Vs2{s}"], writes=["P4"])

            load_group(0)
            for q in range(NSL):
                load_u(q)
                load_v(q)
            gb_prep(0, 0)
            for ci in range(NCH):
                if ci + 1 < NCH:
                    gb_prep(0, ci + 1)
                gb_mm(0, ci)
            for gi in range(NG):
                t0 = gi * TG
                if gi + 1 < NG:
                    load_group(gi + 1)
                    gb_prep(gi + 1, 0)
                for n in range(128 + LOOK):
                    if n < 128:
                        emit_A(gi, n)
                        if n % SBK == SBK - 1:
                            load_u(gi * NSB + n // SBK + NSL)
                        if gi + 1 < NG and n % 2 == 1:
                            ci = n // 2
                            if ci + 1 < NCH:
                                gb_prep(gi + 1, ci + 1)
                            gb_mm(gi + 1, ci)
                    m = n - LOOK
                    if m >= 0:
                        emit_Y(gi, m)
                        if m % SBK == SBK - 1:
                            load_v(gi * NSB + m // SBK + NSL)
                for a in range(2):
                    T.dma("sp", x1g[:], X1[t0 + a * 128:t0 + (a + 1) * 128, :], writes=["x1g"], key="ld_x1g")
                    T.op("dve", lambda e: e.tensor_tensor(out=og[:], in0=yps[:, a * 1024:(a + 1) * 1024], in1=gate2[:], op=ALU.mult),
                         reads=["P4", "gate2"], writes=["og"])
                    T.op("pool", lambda e: e.tensor_tensor(out=og[:], in0=og[:], in1=x1g[:], op=ALU.add),
                         reads=["og", "x1g"], writes=["og"])
                    T.dma("sp", out_own[t0 + a * 128:t0 + (a + 1) * 128, :], og[:], reads=["og"], key="st_og")
            T.drain()
    return nc


def _host_inputs(inp):
    f32 = np.float32
    x = np.asarray(inp["x"], f32)
    c = np.asarray(inp["c"], f32)
    pos = np.asarray(inp["positions"], np.int32)
    U = np.asarray(inp["peer_u"], f32)
    U_arr = np.ascontiguousarray(U.reshape(128, 128, 8, 128).transpose(0, 3, 2, 1)).reshape(16384, 1024)
    sk = np.asarray(inp["sub_keys"], f32)
    skT = np.ascontiguousarray(sk.reshape(16, 128, 128).transpose(2, 0, 1)).reshape(128, 16 * 128)
    fm = lambda v, n: np.ascontiguousarray(np.asarray(v, f32).reshape(n, 128).T)
    shared = {
        "w_ada": np.asarray(inp["w_ada"], f32),
        "bada_fm": fm(inp["b_ada"], 48),
        "bada_row": np.asarray(inp["b_ada"], f32).reshape(1, -1),
        "g1_fm": fm(inp["norm1_g"], 8), "g2_fm": fm(inp["norm2_g"], 8),
        "w_in": np.asarray(inp["w_in"], f32),
        "gqa_fm": fm(inp["mla_qa_g"], 3), "gkva_fm": fm(inp["mla_kva_g"], 2),
        "w_uq": np.asarray(inp["w_uq"], f32), "w_ukv": np.asarray(inp["w_ukv"], f32),
        "gq_row": np.asarray(inp["mla_q_g"], f32).reshape(1, -1), "gk_row": np.asarray(inp["mla_k_g"], f32).reshape(1, -1),
        "gfq_row": np.asarray(inp["fox_q_g"], f32).reshape(1, -1), "gfk_row": np.asarray(inp["fox_k_g"], f32).reshape(1, -1),
        "bf_col": np.asarray(inp["b_f"], f32).reshape(8, 1),
        "w_o": np.asarray(inp["w_o"], f32), "w_pq": np.asarray(inp["w_pq"], f32),
        "skT": skT, "U_arr": U_arr, "V_in": np.asarray(inp["peer_v"], f32),
        "c_ident_bf": np.eye(128, dtype=f32).astype(ml_dtypes.bfloat16),
        "c_ident_f": np.eye(128, dtype=f32),
        "c_invf": np.tile((f32(10000.0) ** (-np.arange(0, 32, 2, dtype=f32) / f32(32))).astype(f32)[None, :], (128, 1)),
        "c_iota": np.tile(np.arange(128, dtype=f32)[None, :], (128, 1)),
    }
    r = np.arange(128)
    tri = np.where(r[:, None] > r[None, :], NEG, 0.0).astype(f32)
    full = np.full((128, 128), NEG, f32)
    zero = np.zeros((128, 128), f32)
    in_maps = []
    for core in range(8):
        b, par = core // 2, core % 2
        xb = x[b]
        x_own = np.ascontiguousarray(xb.reshape(32, 2, 128, D)[:, par]).reshape(SO, D)
        pb = pos[b]
        m = dict(shared)
        m.update({
            "x_all": xb, "x_own": x_own,
            "cT": np.ascontiguousarray(c[b].reshape(8, 128).T),
            "pos_all": np.ascontiguousarray(pb.reshape(64, 128).T),
            "pos_own": np.ascontiguousarray(pb.reshape(32, 2, 128)[:, par].T),
            "c_maskA": (tri if par == 0 else zero).astype(ml_dtypes.bfloat16),
            "c_maskB": (full if par == 0 else tri).astype(ml_dtypes.bfloat16),
            "c_par": np.full((128, 1), float(par), f32),
        })
        in_maps.append(m)
    return in_maps


def kernel(**inputs):
    in_maps = _host_inputs(inputs)
    nc = build_program()
    res = run_bass_kernel_spmd(nc, in_maps, core_ids=list(range(8)))
    out = np.empty((4, S, D), np.float32)
    for core in range(8):
        b, par = core // 2, core % 2
        o = np.asarray(res.results[core]["out_own"], np.float32).reshape(32, 128, D)
        out[b].reshape(32, 2, 128, D)[:, par] = o
    return out
```

```python
import numpy as np
from contextlib import ExitStack
import ml_dtypes
import concourse.bass as bass
import concourse.mybir as mybir
from concourse.bass_utils import run_bass_kernel_spmd

F32 = mybir.dt.float32
BF16 = mybir.dt.bfloat16
I32 = mybir.dt.int32
U32 = mybir.dt.uint32
AF = mybir.ActivationFunctionType
ALU = mybir.AluOpType
AX = mybir.AxisListType

D = 1024
S = 8192
NT = 64
NO = 32
SO = 4096
EPS = 1e-6
MAGIC = 12582912.0
TWO_PI = 6.283185307179586
NEG = -30000.0

STOP_AFTER = None
DEBUG = False


PSUM_KEYS = {"S0", "S1", "S2", "S3", "Q0", "Q1", "P4", "P4b", "kvp", "tpB", "tpK", "ops0", "ops1"}


class Tracker:
    L = 4000

    def __init__(self, nc, es):
        self.nc = nc
        self.es = es
        self.eng = {"pe": nc.tensor, "act": nc.scalar, "dve": nc.vector, "pool": nc.gpsimd, "sp": nc.sync}
        self.cnt = {e: 0 for e in self.eng}
        self.sems = {e: [] for e in self.eng}
        self.dsem = {}
        self.dval = {}
        self.waited = {}
        self.lastw = {}
        self.readers = {}

    def _csem(self, e, k):
        i = k // self.L
        while len(self.sems[e]) <= i:
            self.sems[e].append(self.es.enter_context(self.nc.semaphore(f"p_{e}_{len(self.sems[e])}")))
        return self.sems[e][i]

    def _wait(self, eng, dep):
        if dep[0] == "c":
            _, E, k = dep
            if eng == "pe" and E == "pe":
                return
            key = (eng, "c", E)
            if self.waited.get(key, -1) >= k:
                return
            self.waited[key] = k
            self.eng[eng].wait_ge(self._csem(E, k), k % self.L + 1)
        else:
            _, sid, val = dep
            key = (eng, "d", sid)
            if self.waited.get(key, -1) >= val:
                return
            self.waited[key] = val
            self.eng[eng].wait_ge(self.dsem[sid], val)

    def _deps(self, eng, reads, writes):
        deps = []
        for r in reads:
            if r in self.lastw:
                deps.append(self.lastw[r])
            if r in PSUM_KEYS:
                deps.extend(v for k_, v in self.readers.get(r, {}).items() if k_ != ("c", eng))
        for w in writes:
            if w in self.lastw:
                deps.append(self.lastw[w])
            deps.extend(self.readers.get(w, {}).values())
        for d in deps:
            self._wait(eng, d)

    def _record(self, tok, rkey, reads, writes):
        for r in reads:
            self.readers.setdefault(r, {})[rkey] = tok
        for w in writes:
            self.lastw[w] = tok
            self.readers[w] = {}

    def op(self, e, fn, reads=(), writes=()):
        self._deps(e, reads, writes)
        inst = fn(self.eng[e])
        k = self.cnt[e]
        inst.then_inc(self._csem(e, k), 1)
        self.cnt[e] = k + 1
        self._record(("c", e, k), ("c", e), reads, writes)
        return inst

    def dma(self, q, out, in_, reads=(), writes=(), key=None, **kw):
        self._deps(q, reads, writes)
        if key not in self.dsem:
            self.dsem[key] = self.es.enter_context(self.nc.semaphore(f"d_{key}"))
            self.dval[key] = 0
        self.dval[key] += 16
        self.eng[q].dma_start(out=out, in_=in_, **kw).then_inc(self.dsem[key], 16)
        self._record(("d", key, self.dval[key]), ("d", key), reads, writes)

    def drain(self):
        for e in self.eng:
            for E in self.eng:
                if E != e and self.cnt[E] > 0:
                    self._wait(e, ("c", E, self.cnt[E] - 1))
            for sid, v in self.dval.items():
                self._wait(e, ("d", sid, v))


def build_program():
    nc = bass.Bass("TRN2", target_bir_lowering=False)

    def din(name, shape, dt=F32):
        return nc.dram_tensor(name, list(shape), dt, kind="ExternalInput").ap()

    def dscr(name, shape, dt):
        return nc.dram_tensor(name, list(shape), dt, kind=("ExternalOutput" if DEBUG else "Internal")).ap()

    x_all = din("x_all", [S, D])
    x_own = din("x_own", [SO, D])
    cT = din("cT", [128, 8])
    pos_all = din("pos_all", [128, NT], I32)
    pos_own = din("pos_own", [128, NO], I32)
    w_ada = din("w_ada", [D, 6 * D])
    bada_fm = din("bada_fm", [128, 48])
    bada_row = din("bada_row", [1, 6 * D])
    g1_fm = din("g1_fm", [128, 8])
    g2_fm = din("g2_fm", [128, 8])
    w_in = din("w_in", [D, 2216])
    gqa_fm = din("gqa_fm", [128, 3])
    gkva_fm = din("gkva_fm", [128, 2])
    w_uq = din("w_uq", [384, 768])
    w_ukv = din("w_ukv", [256, 1024])
    gq_row = din("gq_row", [1, 96])
    gk_row = din("gk_row", [1, 96])
    gfq_row = din("gfq_row", [1, 64])
    gfk_row = din("gfk_row", [1, 64])
    bf_col = din("bf_col", [8, 1])
    w_o = din("w_o", [D, D])
    w_pq = din("w_pq", [D, 2048])
    skT = din("skT", [128, 16 * 128])
    U_arr = din("U_arr", [16384, 1024])
    V_in = din("V_in", [16384, 1024])
    c_ident_bf = din("c_ident_bf", [128, 128], BF16)
    c_ident_f = din("c_ident_f", [128, 128])
    c_maskA = din("c_maskA", [128, 128], BF16)
    c_maskB = din("c_maskB", [128, 128], BF16)
    c_invf = din("c_invf", [128, 16])
    c_iota = din("c_iota", [128, 128])
    c_par = din("c_par", [128, 1])
    out_own = nc.dram_tensor("out_own", [SO, D], F32, kind="ExternalOutput").ap()

    KT_mla = dscr("KT_mla", [8, 96, S], BF16)
    QT_mla = dscr("QT_mla", [8, 96, SO], BF16)
    V_mla = dscr("V_mla", [NT, 128, 8, 65], BF16)
    KTF_main = dscr("KTF_main", [8, 64, S], BF16)
    KTF_aug = dscr("KTF_aug", [8, 6, S], BF16)
    QTF_main = dscr("QTF_main", [8, 64, SO], BF16)
    QTF_aug = dscr("QTF_aug", [8, 6, SO], BF16)
    V_fox = dscr("V_fox", [NT, 128, 8, 65], BF16)
    OT = dscr("OT", [D, SO], BF16)
    X1 = dscr("X1", [SO, D], F32)
    H2T = dscr("H2T", [128, 8, SO], BF16)
    ET = dscr("ET", [3, 128, SO], F32)
    G1S = dscr("G1S", [128, D], F32)
    Ubf = dscr("Ubf", [16384, 1024], BF16)
    Vbf = dscr("Vbf", [16384, 1024], BF16)

    def brow(ap2d, lo, n):
        return bass.AP(ap2d.tensor, lo, [[0, 128], [1, n]])

    with ExitStack() as es:
        T = Tracker(nc, es)
        _n = [0]

        def sb(es_, shape, dt, name=None):
            _n[0] += 1
            return es_.enter_context(nc.sbuf_tensor(name or f"t{_n[0]}", list(shape), dt))

        P4 = es.enter_context(nc.psum_tensor("P4", [128, 2048], F32))
        Q4 = es.enter_context(nc.psum_tensor("Q4", [128, 2048], F32))
        SB_ = [Q4[:, i * 512:(i + 1) * 512] for i in range(4)]

        def bfv(ap):
            return ap.bitcast(BF16).rearrange("p (c t) -> p c t", t=128)

        ident_bf = sb(es, [128, 128], BF16, "ident_bf")
        ident_f = sb(es, [128, 128], F32, "ident_f")
        maskA = sb(es, [128, 128], BF16, "maskA")
        maskB = sb(es, [128, 128], BF16, "maskB")
        iota = sb(es, [128, 128], F32, "iota")
        parc = sb(es, [128, 1], F32, "parc")
        A1 = sb(es, [128, 8], F32, "A1")
        B1 = sb(es, [128, 8], F32, "B1")
        A2 = sb(es, [128, 8], F32, "A2")
        B2 = sb(es, [128, 8], F32, "B2")
        gate2 = sb(es, [128, D], F32, "gate2")
        mhalf = sb(es, [128, 8], F32, "mhalf")
        ones_f = sb(es, [128, 64], F32, "ones_f")

        ld = lambda o, i, k, **kw: T.dma("sp", o, i, writes=[k], key="ld_" + k, **kw)
        ld(ident_bf[:], c_ident_bf[:, :], "ident_bf")
        ld(ident_f[:], c_ident_f[:, :], "ident_f")
        ld(maskA[:], c_maskA[:, :], "maskA")
        ld(maskB[:], c_maskB[:, :], "maskB")
        ld(iota[:], c_iota[:, :], "iota")
        ld(parc[:], c_par[:, :], "parc")
        T.op("dve", lambda e: e.memset(mhalf[:], -0.5), writes=["mhalf"])
        T.op("dve", lambda e: e.memset(ones_f[:], 1.0), writes=["ones_f"])

        cast_jobs = []
        for i in range(16):
            cast_jobs.append((Ubf[i * 1024:(i + 1) * 1024, :], U_arr[i * 1024:(i + 1) * 1024, :]))
        for i in range(16):
            cast_jobs.append((Vbf[i * 1024:(i + 1) * 1024, :], V_in[i * 1024:(i + 1) * 1024, :]))

        def issue_cast():
            if cast_jobs:
                o, i = cast_jobs.pop(0)
                T.dma("pool", o, i, key="uvcast")

        def rsqrt_mean(ss_ap, key_ss, out_ap, key_out, n, tmp_ap, key_tmp):
            m = ss_ap.shape[-1]
            T.op("dve", lambda e: e.tensor_scalar(out=tmp_ap, in0=ss_ap, scalar1=1.0 / n, scalar2=EPS,
                                                  op0=ALU.mult, op1=ALU.add), reads=[key_ss], writes=[key_tmp])
            T.op("pool", lambda e: e.tensor_tensor(out=out_ap, in0=tmp_ap, in1=mhalf[:, 0:m], op=ALU.pow),
                 reads=[key_tmp, "mhalf"], writes=[key_out])

        pfl = es.enter_context(ExitStack())
        FLT = sb(pfl, [8, S], F32, "FLT")
        with ExitStack() as pb:
            w_in_sb = sb(pb, [128, 8, 2216], BF16, "w_in_sb")
            w_uq_sb = sb(pb, [128, 3, 768], BF16, "w_uq_sb")
            w_ukv_sb = sb(pb, [128, 2, 1024], BF16, "w_ukv_sb")
            for c in range(8):
                T.dma("pool", w_in_sb[:, c, :], w_in[c * 128:(c + 1) * 128, :], writes=["w_in_sb"], key="ld_w_in",
                      max_dma_last_dim=4096)
            T.dma("pool", w_uq_sb[:], w_uq.rearrange("(c p) n -> p c n", p=128), writes=["w_uq_sb"], key="ld_w_uq")
            T.dma("pool", w_ukv_sb[:], w_ukv.rearrange("(c p) n -> p c n", p=128), writes=["w_ukv_sb"], key="ld_w_ukv")

            gqa = sb(pb, [128, 3], F32, "gqa")
            gkva = sb(pb, [128, 2], F32, "gkva")
            gq_bc = sb(pb, [128, 96], F32, "gq_bc")
            gk_bc = sb(pb, [128, 96], F32, "gk_bc")
            gfq_bc = sb(pb, [128, 64], F32, "gfq_bc")
            gfk_bc = sb(pb, [128, 64], F32, "gfk_bc")
            ld(gqa[:], gqa_fm[:, :], "gqa")
            ld(gkva[:], gkva_fm[:, :], "gkva")
            ld(gq_bc[:], brow(gq_row, 0, 96), "gq_bc")
            ld(gk_bc[:], brow(gk_row, 0, 96), "gk_bc")
            ld(gfq_bc[:], brow(gfq_row, 0, 64), "gfq_bc")
            ld(gfk_bc[:], brow(gfk_row, 0, 64), "gfk_bc")
            T.op("dve", lambda e: e.tensor_scalar(out=gq_bc[:], in0=gq_bc[:], scalar1=96.0 ** -0.5, scalar2=None,
                                                  op0=ALU.mult), reads=["gq_bc"], writes=["gq_bc"])
            T.op("dve", lambda e: e.tensor_scalar(out=gfq_bc[:], in0=gfq_bc[:], scalar1=0.125, scalar2=None,
                                                  op0=ALU.mult), reads=["gfq_bc"], writes=["gfq_bc"])

            with ExitStack() as pa:
                c_sb = sb(pa, [128, 8], F32, "c_sb")
                sc = sb(pa, [128, 8], F32, "sc")
                sc_rep = sb(pa, [128, 8, 128], F32, "sc_rep")
                wa = [sb(pa, [128, 8, 1024], F32, f"wa{i}") for i in range(2)]
                bfm = sb(pa, [128, 48], F32, "bfm")
                modfm = sb(pa, [128, 48], F32, "modfm")
                g1s = sb(pa, [128, 8], F32, "g1s")
                g2s = sb(pa, [128, 8], F32, "g2s")
                bg = sb(pa, [128, 1024], F32, "bg")
                gate1 = sb(pa, [128, D], F32, "gate1")
                ld(c_sb[:], cT[:, :], "c_sb")
                ld(bfm[:], bada_fm[:, :], "bfm")
                ld(g1s[:], g1_fm[:, :], "g1s")
                ld(g2s[:], g2_fm[:, :], "g2s")
                T.op("act", lambda e: e.activation(out=sc[:], in_=c_sb[:], func=AF.Silu), reads=["c_sb"], writes=["sc"])
                T.op("dve", lambda e: e.tensor_copy(out=sc_rep[:], in_=sc[:].unsqueeze(2).to_broadcast([128, 8, 128])),
                     reads=["sc"], writes=["sc_rep"])
                mod_ps = SB_[0]
                w_ada_v = w_ada.rearrange("(c p) n -> p c n", p=128)
                for v in range(6):
                    wv = wa[v % 2]
                    wk = f"wa{v % 2}"
                    T.dma("sp", wv[:], w_ada_v[:, :, v * 1024:(v + 1) * 1024], writes=[wk], key="ld_" + wk)
                    if v in (2, 5):
                        gdst, gk = (gate1, "gate1") if v == 2 else (gate2, "gate2")
                        T.dma("sp", bg[:], brow(bada_row, v * 1024, 1024), writes=["bg"], key="ld_bg")
                        for half in range(2):
                            gp = SB_[1 + half]
                            for kc in range(8):
                                T.op("pe", lambda e, kc=kc, gp=gp, half=half, wv=wv: e.matmul(
                                    gp[:, :], lhsT=sc_rep[:, kc, :], rhs=wv[:, kc, half * 512:(half + 1) * 512],
                                    start=(kc == 0), stop=(kc == 7)), reads=["sc_rep", wk], writes=[f"S{1 + half}"])
                            T.op("dve", lambda e, gp=gp, half=half, gdst=gdst: e.tensor_tensor(
                                out=gdst[:, half * 512:(half + 1) * 512], in0=gp[:, :], in1=bg[:, half * 512:(half + 1) * 512],
                                op=ALU.add), reads=[f"S{1 + half}", "bg"], writes=[gk])
                    else:
                        for oc in range(8):
                            col = v * 8 + oc
                            for kc in range(8):
                                T.op("pe", lambda e, kc=kc, oc=oc, col=col, wv=wv: e.matmul(
                                    mod_ps[:, col:col + 1], lhsT=wv[:, kc, oc * 128:(oc + 1) * 128], rhs=sc[:, kc:kc + 1],
                                    start=(kc == 0), stop=(kc == 7)), reads=["sc", wk], writes=["S0"])
                for (c0_, c1_) in ((0, 16), (24, 40)):
                    T.op("dve", lambda e: e.tensor_tensor(out=modfm[:, c0_:c1_], in0=mod_ps[:, c0_:c1_], in1=bfm[:, c0_:c1_], op=ALU.add),
                         reads=["S0", "bfm"], writes=["modfm"])
                T.op("dve", lambda e: e.scalar_tensor_tensor(out=A1[:], in0=modfm[:, 8:16], scalar=1.0, in1=g1s[:],
                                                             op0=ALU.add, op1=ALU.mult), reads=["modfm", "g1s"], writes=["A1"])
                T.op("dve", lambda e: e.tensor_copy(out=B1[:], in_=modfm[:, 0:8]), reads=["modfm"], writes=["B1"])
                T.op("dve", lambda e: e.scalar_tensor_tensor(out=A2[:], in0=modfm[:, 32:40], scalar=1.0, in1=g2s[:],
                                                             op0=ALU.add, op1=ALU.mult), reads=["modfm", "g2s"], writes=["A2"])
                T.op("dve", lambda e: e.tensor_copy(out=B2[:], in_=modfm[:, 24:32]), reads=["modfm"], writes=["B2"])
                T.dma("sp", G1S[:, :], gate1[:], reads=["gate1"], key="st_gate1")
                T.drain()

            cos_all = sb(pb, [128, NT, 16], F32, "cos_all")
            sin_all = sb(pb, [128, NT, 16], F32, "sin_all")
            cos_own = sb(pb, [128, NO, 16], F32, "cos_own")
            sin_own = sb(pb, [128, NO, 16], F32, "sin_own")
            with ExitStack() as pr:
                invf = sb(pr, [128, 16], F32, "invf")
                ld(invf[:], c_invf[:, :], "invf")
                for (pos_d, n, ctab, stab, nm) in ((pos_all, NT, cos_all, sin_all, "all"), (pos_own, NO, cos_own, sin_own, "own")):
                    pi = sb(pr, [128, n], I32, "pi_" + nm)
                    pf = sb(pr, [128, n], F32, "pf_" + nm)
                    ang = sb(pr, [128, n, 16], F32, "ang_" + nm)
                    t1 = sb(pr, [128, n, 16], F32, "t1_" + nm)
                    t2 = sb(pr, [128, n, 16], F32, "t2_" + nm)
                    ld(pi[:], pos_d[:, :], "pi_" + nm)
                    T.op("dve", lambda e: e.tensor_copy(out=pf[:], in_=pi[:]), reads=["pi_" + nm], writes=["pf_" + nm])
                    T.op("dve", lambda e: e.tensor_tensor(out=ang[:], in0=pf[:].unsqueeze(2).to_broadcast([128, n, 16]),
                                                          in1=invf[:].unsqueeze(1).to_broadcast([128, n, 16]), op=ALU.mult),
                         reads=["pf_" + nm, "invf"], writes=["ang_" + nm])
                    for (tab, tk, shift) in ((stab, "sin_" + nm, 0.0), (ctab, "cos_" + nm, TWO_PI / 4)):
                        T.op("dve", lambda e: e.tensor_scalar(out=t1[:], in0=ang[:], scalar1=shift, scalar2=1.0 / TWO_PI,
                                                              op0=ALU.add, op1=ALU.mult), reads=["ang_" + nm], writes=["t1_" + nm])
                        T.op("dve", lambda e: e.tensor_scalar(out=t2[:], in0=t1[:], scalar1=MAGIC, scalar2=None, op0=ALU.add),
                             reads=["t1_" + nm], writes=["t2_" + nm])
                        T.op("dve", lambda e: e.tensor_scalar(out=t1[:], in0=t2[:], scalar1=-MAGIC, scalar2=-TWO_PI,
                                                              op0=ALU.add, op1=ALU.mult), reads=["t2_" + nm], writes=["t1_" + nm])
                        T.op("dve", lambda e: e.scalar_tensor_tensor(out=t2[:], in0=ang[:], scalar=shift, in1=t1[:],
                                                                     op0=ALU.add, op1=ALU.add), reads=["ang_" + nm, "t1_" + nm],
                             writes=["t2_" + nm])
                        T.op("dve", lambda e: e.tensor_scalar(out=t2[:], in0=t2[:], scalar1=3.1415925, scalar2=-3.1415925,
                                                              op0=ALU.min, op1=ALU.max), reads=["t2_" + nm], writes=["t2_" + nm])
                        T.op("act", lambda e, tab=tab: e.activation(out=tab[:], in_=t2[:], func=AF.Sin),
                             reads=["t2_" + nm], writes=[tk])
                T.drain()

            ones_bf = sb(pb, [1, 128], BF16, "ones_bf")
            brow = sb(pb, [1, 2216], BF16, "brow")
            B1b = sb(pb, [128, 8], BF16, "B1b")
            T.op("dve", lambda e: e.memset(ones_bf[:], 1.0), writes=["ones_bf"])
            T.op("dve", lambda e: e.tensor_copy(out=B1b[:], in_=B1[:]), reads=["B1"], writes=["B1b"])
            for c0_ in range(0, 2216, 512):
                c1_ = min(2216, c0_ + 512)
                for c in range(8):
                    T.op("pe", lambda e: e.matmul(SB_[0][0:1, 0:c1_ - c0_], lhsT=B1b[:, c:c + 1], rhs=w_in_sb[:, c, c0_:c1_],
                                                  start=(c == 0), stop=(c == 7)), reads=["B1b", "w_in_sb"], writes=["S0"])
                T.op("act", lambda e: e.activation(out=brow[0:1, c0_:c1_], in_=SB_[0][0:1, 0:c1_ - c0_], func=AF.Copy),
                     reads=["S0"], writes=["brow"])
            for c in range(8):
                T.op("dve", lambda e: e.tensor_scalar(out=w_in_sb[:, c, :], in0=w_in_sb[:, c, :], scalar1=A1[:, c:c + 1], scalar2=None,
                                                      op0=ALU.mult), reads=["w_in_sb", "A1"], writes=["w_in_sb"])

            xa = [sb(pb, [128, D], F32, f"xa{i}") for i in range(2)]
            xn = sb(pb, [128, D], BF16, "xn")
            hT = sb(pb, [128, 8, 128], BF16, "hT")

            def statset(tag, jw):
                return dict(junk=sb(pb, [128, jw], F32, "junk_" + tag), st=sb(pb, [128, 8], F32, "st_" + tag),
                            st2=sb(pb, [128, 8], F32, "st2_" + tag), rs=sb(pb, [128, 8], F32, "rs_" + tag), tag=tag)
            SX = statset("x", D)
            SC = statset("c", 384)
            SK = statset("k", 1024)
            SF = statset("f", 512)
            junk_p = sb(pb, [128, 32], F32, "junk_p")
            sm = sb(pb, [128, 8], F32, "sm")
            cs = sb(pb, [128, 384], BF16, "cs")
            cT_sb = sb(pb, [128, 3, 128], BF16, "cT_sb")
            kpg = sb(pb, [128, 32], F32, "kpg")
            kr = sb(pb, [128, 32], F32, "kr")
            rtk = sb(pb, [128, 8, 64], F32, "rtk")
            rtq = sb(pb, [128, 8, 64], F32, "rtq")
            qg = sb(pb, [128, 8, 96], F32, "qg")
            qgf = sb(pb, [128, 8, 64], F32, "qgf")
            Kt = sb(pb, [128, 8, 96], BF16, "Kt")
            KTt = sb(pb, [128, 8, 128], BF16, "KTt")
            Vt = [sb(pb, [128, 8, 65], BF16, f"Vt{i}") for i in range(2)]
            Ft = sb(pb, [128, 8, 64], BF16, "Ft")
            FTt = sb(pb, [128, 4, 128], BF16, "FTt")
            for i in range(2):
                T.op("dve", lambda e, i=i: e.memset(Vt[i][:], 1.0), writes=[f"Vt{i}"])

            tp = bfv(SB_[0][:, :])
            pj = [SB_[1], SB_[2], SB_[3]]
            kvp = P4[:, 0:1024]
            tpB = bfv(P4[:, 1024:1536])
            tpK = bfv(P4[:, 1536:2048])
            vcnt = [0]

            def rsq(SS, m, n):
                tg = SS["tag"]
                T.op("act", lambda e: e.activation(out=SS["st2"][:, 0:m], in_=SS["st"][:, 0:m], func=AF.Sqrt, scale=1.0 / n, bias=EPS),
                     reads=["st_" + tg], writes=["st2_" + tg])
                T.op("dve", lambda e: e.reciprocal(out=SS["rs"][:, 0:m], in_=SS["st2"][:, 0:m]),
                     reads=["st2_" + tg], writes=["rs_" + tg])

            def norm_T(xt, xk, Aa, Bb, Ak, Bk):
                T.op("act", lambda e: e.activation(out=SX["junk"][:], in_=xt[:], func=AF.Square, accum_out=SX["st"][:, 0:1]),
                     reads=[xk], writes=["junk_x", "st_x"])
                rsq(SX, 1, float(D))
                T.op("dve", lambda e: e.tensor_scalar(out=xn[:], in0=xt[:], scalar1=SX["rs"][:, 0:1], scalar2=None, op0=ALU.mult),
                     reads=[xk, "rs_x"], writes=["xn"])
                for c in range(8):
                    T.op("pe", lambda e: e.transpose(out=tp[:, c, :], in_=xn[:, c * 128:(c + 1) * 128], identity=ident_bf[:]),
                         reads=["xn", "ident_bf"], writes=["S0"])
                T.op("act", lambda e: e.activation(out=hT[:], in_=tp[:, :, :], func=AF.Copy), reads=["S0"], writes=["hT"])

            def proj(ps, pk, c0, c1):
                for c in range(8):
                    T.op("pe", lambda e: e.matmul(ps[:, 0:c1 - c0], lhsT=hT[:, c, :], rhs=w_in_sb[:, c, c0:c1],
                                                  start=(c == 0), stop=False), reads=["hT", "w_in_sb"], writes=[pk])
                T.op("pe", lambda e: e.matmul(ps[:, 0:c1 - c0], lhsT=ones_bf[0:1, :], rhs=brow[0:1, c0:c1], start=False, stop=True),
                     reads=["ones_bf", "brow"], writes=[pk])

            def lowrank(ps_ap, pk, n, gfm, gk, wsb, wk, outw):
                width = n * 128
                T.op("act", lambda e: e.activation(out=SC["junk"][:, 0:width], in_=ps_ap, func=AF.Square, accum_out=SC["st"][:, 0:1]),
                     reads=[pk], writes=["junk_c", "st_c"])
                rsq(SC, 1, float(width))
                T.op("dve", lambda e: e.tensor_scalar(out=cs[:, 0:width], in0=ps_ap, scalar1=SC["rs"][:, 0:1], scalar2=None,
                                                      op0=ALU.mult), reads=[pk, "rs_c"], writes=["cs"])
                for c in range(n):
                    T.op("pe", lambda e: e.transpose(out=tpB[:, c, :], in_=cs[:, c * 128:(c + 1) * 128], identity=ident_bf[:]),
                         reads=["cs", "ident_bf"], writes=["tpB"])
                for c in range(n):
                    T.op("act", lambda e: e.activation(out=cT_sb[:, c, :], in_=tpB[:, c, :], func=AF.Copy,
                                                       scale=gfm[:, c:c + 1]), reads=["tpB", gk], writes=["cT_sb"])
                for h0 in range(0, outw, 512):
                    h1 = min(outw, h0 + 512)
                    for c in range(n):
                        T.op("pe", lambda e: e.matmul(kvp[:, h0:h1], lhsT=cT_sb[:, c, :], rhs=wsb[:, c, h0:h1],
                                                      start=(c == 0), stop=(c == n - 1)),
                             reads=["cT_sb", wk], writes=["kvp"])

            def rope(src, dst, cosb, sinb, ck, sk_, srck, dstk, nh, rt, rtk_):
                shp = [128, nh, 16]
                cb = cosb.unsqueeze(1).to_broadcast(shp)
                sbb = sinb.unsqueeze(1).to_broadcast(shp)
                x1 = src[:, :, 0:16]
                x2 = src[:, :, 16:32]
                r = [rt[:, 0:nh, i * 16:(i + 1) * 16] for i in range(4)]
                T.op("pool", lambda e: e.tensor_tensor(out=r[0], in0=x1, in1=cb, op=ALU.mult), reads=[srck, ck], writes=[rtk_ + "0"])
                T.op("pool", lambda e: e.tensor_tensor(out=r[1], in0=x2, in1=sbb, op=ALU.mult), reads=[srck, sk_], writes=[rtk_ + "1"])
                T.op("pool", lambda e: e.tensor_tensor(out=r[2], in0=x2, in1=cb, op=ALU.mult), reads=[srck, ck], writes=[rtk_ + "2"])
                T.op("pool", lambda e: e.tensor_tensor(out=r[3], in0=x1, in1=sbb, op=ALU.mult), reads=[srck, sk_], writes=[rtk_ + "3"])
                T.op("pool", lambda e: e.tensor_tensor(out=dst[:, :, 0:16], in0=r[0], in1=r[1], op=ALU.subtract),
                     reads=[rtk_ + "0", rtk_ + "1"], writes=[dstk])
                T.op("pool", lambda e: e.tensor_tensor(out=dst[:, :, 16:32], in0=r[2], in1=r[3], op=ALU.add),
                     reads=[rtk_ + "2", rtk_ + "3"], writes=[dstk])

            def head_sumsq(SS, ps_ap, pk, width, nh, dh, lo, hi):
                tg = SS["tag"]
                T.op("act", lambda e: e.activation(out=SS["junk"][:, 0:width], in_=ps_ap, func=AF.Square), reads=[pk], writes=["junk_" + tg])
                jv = SS["junk"][:, 0:width].rearrange("p (h d) -> p h d", d=dh)[:, :, lo:hi]
                T.op("dve", lambda e: e.tensor_reduce(out=SS["st"][:, 0:nh], in_=jv, axis=AX.X, op=ALU.add),
                     reads=["junk_" + tg], writes=["st_" + tg])

            def fox_qk(ps, pk, gbc, gk, dstT_main, col0):
                head_sumsq(SF, ps[:, :], pk, 512, 8, 64, 0, 64)
                rsq(SF, 8, 64.0)
                pv = ps[:, :].rearrange("p (h d) -> p h d", d=64)
                T.op("dve", lambda e: e.tensor_tensor(out=qgf[:], in0=pv, in1=gbc[:].unsqueeze(1).to_broadcast([128, 8, 64]),
                                                      op=ALU.mult), reads=[pk, gk], writes=["qgf"])
                T.op("pool", lambda e: e.tensor_tensor(out=Ft[:], in0=qgf[:], in1=SF["rs"][:, 0:8].unsqueeze(2).to_broadcast([128, 8, 64]),
                                                       op=ALU.mult), reads=["qgf", "rs_f"], writes=["Ft"])
                fv_ = Ft[:].rearrange("p (q two) d -> p q (two d)", two=2)
                for q in range(4):
                    T.op("pe", lambda e: e.transpose(out=tpB[:, 4 + q, :], in_=fv_[:, q, :], identity=ident_bf[:]),
                         reads=["Ft", "ident_bf"], writes=["tpB"])
                T.op("act", lambda e: e.activation(out=FTt[:], in_=tpB[:, 4:8, :], func=AF.Copy), reads=["tpB"], writes=["FTt"])
                dv = dstT_main.rearrange("(q two) d t -> (two d) q t", two=2)[:, :, col0:col0 + 128]
                T.dma("sp", dv, FTt[:], reads=["FTt"], key="st_FTt")

            def kv_front(ti, ui):
                s = ui % 2
                xt, xk = xa[s], f"xa{s}"
                norm_T(xt, xk, A1, B1, "A1", "B1")
                proj(pj[0], "S1", 384, 672)
                for c in range(8):
                    T.op("pe", lambda e: e.matmul(pj[0][0:8, 384:512], lhsT=w_in_sb[:, c, 2208:2216], rhs=hT[:, c, :],
                                                  start=(c == 0), stop=False), reads=["hT", "w_in_sb"], writes=["S1"])
                T.op("pe", lambda e: e.matmul(pj[0][0:8, 384:512], lhsT=brow[0:1, 2208:2216], rhs=ones_bf[0:1, :], start=False, stop=True),
                     reads=["ones_bf", "brow"], writes=["S1"])
                proj(pj[1], "S2", 1184, 1696)
                proj(pj[2], "S3", 1696, 2208)

            def kv_mid(ti):
                T.op("act", lambda e: e.activation(out=FLT[:, ti * 128:(ti + 1) * 128], in_=pj[0][0:8, 384:512], func=AF.Copy),
                     reads=["S1"], writes=["FLT"])
                T.op("act", lambda e: e.activation(out=junk_p[:], in_=pj[0][:, 256:288], func=AF.Square, accum_out=sm[:, 0:1]),
                     reads=["S1"], writes=["junk_p", "sm"])
                T.op("dve", lambda e: e.tensor_tensor(out=kpg[:], in0=pj[0][:, 256:288], in1=gk_bc[:, 64:96], op=ALU.mult),
                     reads=["S1", "gk_bc"], writes=["kpg"])
                lowrank(pj[0][:, 0:256], "S1", 2, gkva, "gkva", w_ukv_sb, "w_ukv_sb", 1024)
                rope(kpg[:].unsqueeze(1), kr[:].unsqueeze(1), cos_all[:, ti, :], sin_all[:, ti, :], "cos_all", "sin_all", "kpg", "kr", 1,
                     rtk, "rtk")
                vs = vcnt[0] % 2
                vcnt[0] += 1
                T.op("act", lambda e: e.activation(out=Vt[vs][:, :, 0:64], in_=pj[2][:, :].rearrange("p (h d) -> p h d", d=64),
                                                   func=AF.Copy), reads=["S3"], writes=[f"Vt{vs}"])
                T.dma("sp", V_fox[ti], Vt[vs][:], reads=[f"Vt{vs}"], key=f"st_Vt{vs}")
                fox_qk(pj[1], "S2", gfk_bc, "gfk_bc", KTF_main, ti * 128)

            def kv_tail(ti):
                head_sumsq(SK, kvp, "kvp", 1024, 8, 128, 0, 64)
                T.op("dve", lambda e: e.tensor_scalar(out=SK["st"][:, 0:8], in0=SK["st"][:, 0:8], scalar1=sm[:, 0:1], scalar2=None, op0=ALU.add),
                     reads=["st_k", "sm"], writes=["st_k"])
                rsq(SK, 8, 96.0)
                kvv = kvp.rearrange("p (h d) -> p h d", d=128)
                vs = vcnt[0] % 2
                vcnt[0] += 1
                T.op("act", lambda e: e.activation(out=Vt[vs][:, :, 0:64], in_=kvv[:, :, 64:128], func=AF.Copy),
                     reads=["kvp"], writes=[f"Vt{vs}"])
                T.dma("sp", V_mla[ti], Vt[vs][:], reads=[f"Vt{vs}"], key=f"st_Vt{vs}")
                T.op("dve", lambda e: e.tensor_tensor(out=qg[:, :, 0:64], in0=kvv[:, :, 0:64],
                                                      in1=gk_bc[:, 0:64].unsqueeze(1).to_broadcast([128, 8, 64]), op=ALU.mult),
                     reads=["kvp", "gk_bc"], writes=["qg"])
                T.op("dve", lambda e: e.tensor_tensor(out=Kt[:, :, 0:64], in0=qg[:, :, 0:64],
                                                      in1=SK["rs"][:, 0:8].unsqueeze(2).to_broadcast([128, 8, 64]), op=ALU.mult),
                     reads=["qg", "rs_k"], writes=["Kt"])
                T.op("pool", lambda e: e.tensor_tensor(out=Kt[:, :, 64:96], in0=kr[:].unsqueeze(1).to_broadcast([128, 8, 32]),
                                                       in1=SK["rs"][:, 0:8].unsqueeze(2).to_broadcast([128, 8, 32]), op=ALU.mult),
                     reads=["kr", "rs_k"], writes=["Kt"])
                for h in range(8):
                    T.op("pe", lambda e: e.transpose(out=tpK[0:96, h, :], in_=Kt[:, h, :], identity=ident_bf[:]),
                         reads=["Kt", "ident_bf"], writes=["tpK"])
                T.op("act", lambda e: e.activation(out=KTt[0:96, :, :], in_=tpK[0:96, :, :], func=AF.Copy), reads=["tpK"], writes=["KTt"])
                T.dma("sp", KT_mla[:, :, ti * 128:(ti + 1) * 128].rearrange("h d t -> d h t"), KTt[0:96, :, :], reads=["KTt"], key="st_KTt")

            def q_front(k, ui):
                s = ui % 2
                xt, xk = xa[s], f"xa{s}"
                norm_T(xt, xk, A1, B1, "A1", "B1")
                proj(pj[0], "S1", 0, 384)
                proj(pj[1], "S2", 672, 1184)

            def q_mid(k):
                lowrank(pj[0][:, 0:384], "S1", 3, gqa, "gqa", w_uq_sb, "w_uq_sb", 768)
                fox_qk(pj[1], "S2", gfq_bc, "gfq_bc", QTF_main, k * 128)

            def q_tail(k):
                qp_ = kvp[:, 0:768]
                head_sumsq(SK, qp_, "kvp", 768, 8, 96, 0, 96)
                rsq(SK, 8, 96.0)
                T.op("dve", lambda e: e.tensor_tensor(out=qg[:], in0=qp_.rearrange("p (h d) -> p h d", d=96),
                                                      in1=gq_bc[:].unsqueeze(1).to_broadcast([128, 8, 96]), op=ALU.mult),
                     reads=["kvp", "gq_bc"], writes=["qg"])
                rope(qg[:, :, 64:96], qg[:, :, 64:96], cos_own[:, k, :], sin_own[:, k, :], "cos_own", "sin_own", "qg", "qg", 8, rtq, "rtq")
                T.op("dve", lambda e: e.tensor_tensor(out=Kt[:], in0=qg[:], in1=SK["rs"][:, 0:8].unsqueeze(2).to_broadcast([128, 8, 96]),
                                                      op=ALU.mult), reads=["qg", "rs_k"], writes=["Kt"])
                for h in range(8):
                    T.op("pe", lambda e: e.transpose(out=tpK[0:96, h, :], in_=Kt[:, h, :], identity=ident_bf[:]),
                         reads=["Kt", "ident_bf"], writes=["tpK"])
                T.op("act", lambda e: e.activation(out=KTt[0:96, :, :], in_=tpK[0:96, :, :], func=AF.Copy), reads=["tpK"], writes=["KTt"])
                T.dma("sp", QT_mla[:, :, k * 128:(k + 1) * 128].rearrange("h d t -> d h t"), KTt[0:96, :, :], reads=["KTt"], key="st_KTt")

            units = []
            for k in range(NO):
                units.append((kv_front, kv_mid, kv_tail, 2 * k))
                units.append((kv_front, kv_mid, kv_tail, 2 * k + 1))
                units.append((q_front, q_mid, q_tail, k))
            def load_x(ui):
                fr_, _, _, arg_ = units[ui]
                src = x_all if fr_ is kv_front else x_own
                T.dma("sp", xa[ui % 2][:], src[arg_ * 128:(arg_ + 1) * 128, :], writes=[f"xa{ui % 2}"], key=f"ld_xa{ui % 2}")

            prev_u = None
            load_x(0)
            for ui, (fr, md, tl, arg) in enumerate(units):
                if ui % 3 == 0:
                    issue_cast()
                if ui + 1 < len(units):
                    load_x(ui + 1)
                fr(arg, ui)
                if prev_u is not None:
                    prev_u[0](prev_u[1])
                md(arg)
                prev_u = (tl, arg)
            prev_u[0](prev_u[1])
            while cast_jobs:
                issue_cast()

            T.drain()

        with ExitStack() as pf_:
            bfc = sb(pf_, [8, 1], F32, "bfc")
            nbf = sb(pf_, [8, 1], F32, "nbf")
            w1 = sb(pf_, [8, S], F32, "w1")
            fo = sb(pf_, [8, SO], F32, "fo")
            aug = sb(pf_, [8, 3, S], BF16, "aug")
            onesb = sb(pf_, [8, 3, 2048], BF16, "onesb")
            ld(bfc[:], bf_col[:, :], "bfc")
            T.op("dve", lambda e: e.tensor_scalar(out=nbf[:], in0=bfc[:], scalar1=-1.0, scalar2=None, op0=ALU.mult),
                 reads=["bfc"], writes=["nbf"])
            T.op("dve", lambda e: e.memset(onesb[:], 1.0), writes=["onesb"])
            for q in range(4):
                T.dma("sp", KTF_aug[:, 0:3, q * 2048:(q + 1) * 2048], onesb[:], reads=["onesb"], key="st_onesb")
            for q in range(2):
                T.dma("sp", QTF_aug[:, 3:6, q * 2048:(q + 1) * 2048], onesb[:], reads=["onesb"], key="st_onesb")
            T.op("act", lambda e: e.activation(out=w1[:], in_=FLT[:], func=AF.Exp, bias=nbf[:, 0:1], scale=-1.0),
                 reads=["FLT", "nbf"], writes=["w1"])
            T.op("act", lambda e: e.activation(out=w1[:], in_=w1[:], func=AF.Ln, bias=1.0, scale=1.0), reads=["w1"], writes=["w1"])
            T.op("dve", lambda e: e.tensor_scalar(out=w1[:], in0=w1[:], scalar1=-1.0, scalar2=None, op0=ALU.mult),
                 reads=["w1"], writes=["w1"])
            T.op("dve", lambda e: e.tensor_tensor_scan(out=FLT[:], data0=ones_f[0:8, 0:1].to_broadcast([8, S]), data1=w1[:],
                                                       initial=0.0, op0=ALU.mult, op1=ALU.add), reads=["w1", "ones_f"], writes=["FLT"])

            def split3(src, srck, n, sign, tmpa, tmpk):
                cur, curk = src, srck
                for i in range(3):
                    sg = sign if i == 0 else 1.0
                    T.op("dve", lambda e: e.tensor_scalar(out=aug[:, i, 0:n], in0=cur, scalar1=sg, scalar2=None, op0=ALU.mult),
                         reads=[curk], writes=["aug"])
                    if i < 2:
                        T.op("dve", lambda e: e.scalar_tensor_tensor(out=tmpa, in0=cur, scalar=sg, in1=aug[:, i, 0:n],
                                                                     op0=ALU.mult, op1=ALU.subtract), reads=[curk, "aug"], writes=[tmpk])
                        cur, curk = tmpa, tmpk

            split3(FLT[:], "FLT", S, -1.0, w1[:], "w1")
            T.dma("sp", KTF_aug[:, 3:6, :], aug[:], reads=["aug"], key="st_aug")
            fv4 = FLT[:].rearrange("p (k two r) -> p k two r", two=2, r=128)
            fo3 = fo[:].rearrange("p (k r) -> p k r", r=128)
            T.op("dve", lambda e: e.tensor_tensor(out=fo3, in0=fv4[:, :, 1, :], in1=fv4[:, :, 0, :], op=ALU.subtract),
                 reads=["FLT"], writes=["fo"])
            T.op("dve", lambda e: e.scalar_tensor_tensor(out=fo3, in0=fo3, scalar=parc[0:8, 0:1], in1=fv4[:, :, 0, :],
                                                         op0=ALU.mult, op1=ALU.add), reads=["fo", "parc", "FLT"], writes=["fo"])
            split3(fo[:], "fo", SO, 1.0, w1[:, 0:SO], "w1")
            T.dma("sp", QTF_aug[:, 0:3, :], aug[:, :, 0:SO], reads=["aug"], key="st_aug")
            T.drain()
        pfl.close()

        if STOP_AFTER == "B":
            T.drain()
            return nc

        with ExitStack() as pc:
            KTs = [sb(pc, [96, S], BF16, f"KTs{i}") for i in range(2)]
            QTs = [sb(pc, [96, SO], BF16, f"QTs{i}") for i in range(2)]
            Vs = [sb(pc, [128, NT, 65], BF16, f"Vs{i}") for i in range(2)]
            pT = [sb(pc, [128, 2, 512], BF16, f"pT{i}") for i in range(3)]
            osb = [sb(pc, [64, 512], F32, f"osb{i}") for i in range(2)]
            rrow = sb(pc, [1, 512], F32, "rrow")
            rr2 = sb(pc, [33, 512], BF16, "rr2")
            ones33 = sb(pc, [33, 64], BF16, "ones33")
            T.op("dve", lambda e: e.memset(rr2[:], 0.0), writes=["rr2"])
            T.op("dve", lambda e: e.memset(ones33[:], 1.0), writes=["ones33"])
            oT = [sb(pc, [64, 512], BF16, f"oT{i}") for i in range(2)]
            s2 = [Q4[:, 0:1024].rearrange("p (b c) -> p b c", c=512), Q4[:, 1024:2048].rearrange("p (b c) -> p b c", c=512)]
            ops_ = [P4[:, 0:512], P4[:, 512:1024]]
            bcp = P4[:, 1024:1536]

            def load_head(hh):
                s = hh % 2
                if hh < 8:
                    T.dma("sp", KTs[s][0:96, :], KT_mla[hh], writes=[f"KTs{s}"], key=f"ld_KTs{s}")
                    T.dma("sp", QTs[s][0:96, :], QT_mla[hh], writes=[f"QTs{s}"], key=f"ld_QTs{s}")
                    T.dma("sp", Vs[s][:], V_mla[:, :, hh, :].rearrange("t p e -> p t e"), writes=[f"Vs{s}"], key=f"ld_Vs{s}")
                else:
                    h = hh - 8
                    T.dma("sp", KTs[s][0:64, :], KTF_main[h], writes=[f"KTs{s}"], key=f"ld_KTs{s}")
                    T.dma("sp", KTs[s][64:70, :], KTF_aug[h], writes=[f"KTs{s}"], key=f"ld_KTs{s}")
                    T.dma("sp", QTs[s][0:64, :], QTF_main[h], writes=[f"QTs{s}"], key=f"ld_QTs{s}")
                    T.dma("sp", QTs[s][64:70, :], QTF_aug[h], writes=[f"QTs{s}"], key=f"ld_QTs{s}")
                    T.dma("sp", Vs[s][:], V_fox[:, :, h, :].rearrange("t p e -> p t e"), writes=[f"Vs{s}"], key=f"ld_Vs{s}")

            step = [0]
            unit = [0]
            pending_norm = [None]

            def emit_S(hh, g, m):
                s = hh % 2
                dk = 96 if hh < 8 else 70
                n = step[0]
                sp_ = s2[n % 2]
                spk = f"Q{n % 2}"
                jj0 = 2 * m - 8 * g
                diag = jj0 >= 0
                kq = jj0 // 2 if diag else 0
                c0 = kq * 128
                for b_ in range(2):
                    j = 2 * m + b_
                    T.op("pe", lambda e: e.matmul(sp_[:, b_, c0:512], lhsT=KTs[s][0:dk, j * 128:(j + 1) * 128],
                                                  rhs=QTs[s][0:dk, g * 512 + c0:(g + 1) * 512], start=True, stop=not diag),
                         reads=[f"KTs{s}", f"QTs{s}"], writes=[spk])
                    if diag:
                        mk, mkk = (maskA, "maskA") if b_ == 0 else (maskB, "maskB")
                        T.op("pe", lambda e: e.matmul(sp_[:, b_, kq * 128:(kq + 1) * 128], lhsT=ident_bf[:], rhs=mk[:],
                                                      start=False, stop=True), reads=["ident_bf", mkk], writes=[spk])
                return (n, c0)

            def emit_exp_pv(hh, g, m, n, c0, first, last):
                s = hh % 2
                sp_ = s2[n % 2]
                pt = pT[n % 3]
                u = unit[0]
                op_ = ops_[u % 2]
                T.op("act", lambda e: e.activation(out=pt[:, :, c0:512], in_=sp_[:, :, c0:512], func=AF.Exp),
                     reads=[f"Q{n % 2}"], writes=[f"pT{n % 3}"])
                for b_ in range(2):
                    j = 2 * m + b_
                    T.op("pe", lambda e: e.matmul(op_[0:65, c0:512], lhsT=Vs[s][:, j, :], rhs=pt[:, b_, c0:512],
                                                  start=(first and b_ == 0), stop=(last and b_ == 1)),
                         reads=[f"Vs{s}", f"pT{n % 3}"], writes=[f"ops{u % 2}"])

            def emit_norm_a(hh, g, u):
                ob = osb[u % 2]
                T.op("act", lambda e: e.activation(out=ob[:], in_=ops_[u % 2][0:64, :], func=AF.Copy),
                     reads=[f"ops{u % 2}"], writes=[f"osb{u % 2}"])
                T.op("dve", lambda e: e.reciprocal(out=rrow[0:1, :], in_=ops_[u % 2][64:65, :]), reads=[f"ops{u % 2}"], writes=["rrow"])
                T.op("dve", lambda e: e.tensor_copy(out=rr2[0:1, :], in_=rrow[0:1, :]), reads=["rrow"], writes=["rr2"])
                T.op("dve", lambda e: e.tensor_tensor(out=rr2[32:33, :], in0=rrow[0:1, :], in1=rr2[0:1, :], op=ALU.subtract),
                     reads=["rrow", "rr2"], writes=["rr2"])

            def emit_norm_b(hh, g, u):
                ob = osb[u % 2]
                T.op("pe", lambda e: e.matmul(bcp[0:64, :], lhsT=ones33[:, :], rhs=rr2[:, :], start=True, stop=True),
                     reads=["ones33", "rr2"], writes=["P4b"])
                T.op("dve", lambda e: e.tensor_tensor(out=oT[u % 2][:], in0=ob[0:64, :], in1=bcp[0:64, :], op=ALU.mult),
                     reads=[f"osb{u % 2}", "P4b"], writes=[f"oT{u % 2}"])
                T.dma("sp", OT[hh * 64:(hh + 1) * 64, g * 512:(g + 1) * 512], oT[u % 2][:], reads=[f"oT{u % 2}"], key=f"st_oT{u % 2}")

            load_head(0)
            for hh in range(16):
                if hh + 1 < 16:
                    load_head(hh + 1)
                for g in range(8):
                    npair = 4 * g + 4
                    prev = None
                    for m in range(npair):
                        cur = emit_S(hh, g, m)
                        step[0] += 1
                        if prev is not None:
                            emit_exp_pv(hh, g, m - 1, prev[0], prev[1], m - 1 == 0, False)
                        if m == min(5, npair - 1) and pending_norm[0] is not None:
                            emit_norm_b(*pending_norm[0])
                            pending_norm[0] = None
                        prev = cur
                    emit_exp_pv(hh, g, npair - 1, prev[0], prev[1], False, True)
                    emit_norm_a(hh, g, unit[0])
                    pending_norm[0] = (hh, g, unit[0])
                    unit[0] += 1
            emit_norm_b(*pending_norm[0])
            T.drain()

        if STOP_AFTER == "C":
            return nc

        with ExitStack() as pd:
            w_o_sb = sb(pd, [128, 8, D], BF16, "w_o_sb")
            w_pq_sb = sb(pd, [128, 8, 2048], BF16, "w_pq_sb")
            skT_sb = sb(pd, [128, 16, 128], F32, "skT_sb")
            T.dma("pool", w_o_sb[:], w_o.rearrange("(c p) n -> p c n", p=128), writes=["w_o_sb"], key="ld_w_o")
            for c in range(8):
                T.dma("pool", w_pq_sb[:, c, :], w_pq[c * 128:(c + 1) * 128, :], writes=["w_pq_sb"], key="ld_w_pq",
                      max_dma_last_dim=4096)
            ld(skT_sb[:], skT.rearrange("p (a n) -> p a n", n=128), "skT_sb")
            with ExitStack() as pg:
                gate1 = sb(pg, [128, D], F32, "gate1d")
                ld(gate1[:], G1S[:, :], "gate1d")
                T.op("dve", lambda e: e.tensor_tensor(out=w_o_sb[:], in0=w_o_sb[:], in1=gate1[:].unsqueeze(1).to_broadcast([128, 8, D]),
                                                      op=ALU.mult), reads=["w_o_sb", "gate1d"], writes=["w_o_sb"])
                T.drain()
            oTs = [sb(pd, [128, 8, 128], BF16, f"oTs{i}") for i in range(2)]
            xo = [sb(pd, [128, D], F32, f"xo{i}") for i in range(2)]
            x1 = sb(pd, [128, D], F32, "x1")
            junk = sb(pd, [128, D], F32, "junkd")
            xn = sb(pd, [128, D], BF16, "xnd")
            hT = sb(pd, [128, 8, 128], BF16, "hTd")
            st = sb(pd, [128, 8], F32, "std")
            st2 = sb(pd, [128, 8], F32, "st2d")
            rs = sb(pd, [128, 8], F32, "rsd")
            qpT = sb(pd, [128, 16, 128], F32, "qpT")
            scs2 = [sb(pd, [128, 16, 128], F32, f"scs{i}") for i in range(2)]
            scw = sb(pd, [128, 16, 128], F32, "scw")
            top = sb(pd, [128, 16, 16], F32, "top")
            tix = sb(pd, [128, 16, 16], U32, "tix")
            tixf = sb(pd, [128, 16, 16], F32, "tixf")
            cand = sb(pd, [128, 8, 256], F32, "cand")
            candw = sb(pd, [128, 8, 256], F32, "candw")
            ts = sb(pd, [128, 8, 16], F32, "ts")
            tc = sb(pd, [128, 8, 16], U32, "tc")
            tca = sb(pd, [128, 8, 16], U32, "tca")
            tcb = sb(pd, [128, 8, 16], U32, "tcb")
            taf = sb(pd, [128, 8, 16], F32, "taf")
            tbf = sb(pd, [128, 8, 16], F32, "tbf")
            ohs = [[sb(pd, [128, 8, 16, 16], F32, f"oh{i}{j}") for j in range(2)] for i in range(2)]
            ees = [sb(pd, [128, 3, 128], F32, f"ee{i}") for i in range(2)]
            eT = sb(pd, [128, 3, 128], F32, "eT")
            gsum = sb(pd, [128, 8], F32, "gsum")
            tp = bfv(SB_[0][:, :])
            scp = P4

            def norm_T2(xt, xk):
                T.op("act", lambda e: e.activation(out=junk[:], in_=xt[:], func=AF.Square, accum_out=st[:, 0:1]),
                     reads=[xk], writes=["junkd", "std"])
                T.op("pool", lambda e: e.tensor_scalar(out=st2[:, 0:1], in0=st[:, 0:1], scalar1=1.0 / D, scalar2=EPS,
                                                       op0=ALU.mult, op1=ALU.add), reads=["std"], writes=["st2d"])
                T.op("pool", lambda e: e.tensor_tensor(out=rs[:, 0:1], in0=st2[:, 0:1], in1=mhalf[:, 0:1], op=ALU.pow),
                     reads=["st2d", "mhalf"], writes=["rsd"])
                T.op("act", lambda e: e.activation(out=xn[:], in_=xt[:], func=AF.Copy, scale=rs[:, 0:1]),
                     reads=[xk, "rsd"], writes=["xnd"])
                for c in range(8):
                    T.op("pe", lambda e, c=c: e.transpose(out=tp[:, c, :], in_=xn[:, c * 128:(c + 1) * 128], identity=ident_bf[:]),
                         reads=["xnd", "ident_bf"], writes=["S0"])
                for c in range(8):
                    T.op("act", lambda e, c=c: e.activation(out=hT[:, c, :], in_=tp[:, c, :], func=AF.Identity,
                                                           bias=B2[:, c:c + 1], scale=A2[:, c:c + 1]),
                         reads=["S0", "A2", "B2"], writes=["hTd"])

            def top16(src3, srck, work3, workk, nseg, width, vals, valk, idx, idxk):
                for s_ in range(nseg):
                    T.op("dve", lambda e, s_=s_: e.max(out=vals[:, s_, 0:8], in_=src3[:, s_, :]), reads=[srck], writes=[valk])
                    T.op("dve", lambda e, s_=s_: e.match_replace(out=work3[:, s_, :], in_to_replace=vals[:, s_, 0:8],
                                                                in_values=src3[:, s_, :], imm_value=-1e30),
                         reads=[srck, valk], writes=[workk])
                    T.op("dve", lambda e, s_=s_: e.max(out=vals[:, s_, 8:16], in_=work3[:, s_, :]), reads=[workk], writes=[valk])
                    T.op("dve", lambda e, s_=s_: e.max_index(out=idx[:, s_, 0:8], in_max=vals[:, s_, 0:8], in_values=src3[:, s_, :]),
                         reads=[srck, valk], writes=[idxk])
                    T.op("dve", lambda e, s_=s_: e.max_index(out=idx[:, s_, 8:16], in_max=vals[:, s_, 8:16], in_values=src3[:, s_, :]),
                         reads=[srck, valk], writes=[idxk])

            def d_load(k):
                s = k % 2
                T.dma("sp", oTs[s][:], OT[:, k * 128:(k + 1) * 128].rearrange("(c p) t -> p c t", p=128), writes=[f"oTs{s}"],
                      key=f"ld_oTs{s}")
                T.dma("sp", xo[s][:], x_own[k * 128:(k + 1) * 128, :], writes=[f"xo{s}"], key=f"ld_xo{s}")

            def d_front(k):
                s = k % 2
                for half in range(2):
                    for c in range(8):
                        T.op("pe", lambda e, c=c, half=half: e.matmul(SB_[1 + half][:, :], lhsT=oTs[s][:, c, :],
                                                                     rhs=w_o_sb[:, c, half * 512:(half + 1) * 512],
                                                                     start=(c == 0), stop=(c == 7)),
                             reads=[f"oTs{s}", "w_o_sb"], writes=[f"S{1 + half}"])
                for half in range(2):
                    sl = slice(half * 512, (half + 1) * 512)
                    T.op("act", lambda e, half=half, sl=sl: e.activation(out=x1[:, sl], in_=SB_[1 + half][:, :], func=AF.Copy),
                         reads=[f"S{1 + half}"], writes=["x1"])
                T.op("pool", lambda e: e.tensor_tensor(out=x1[:], in0=x1[:], in1=xo[s][:], op=ALU.add),
                     reads=["x1", f"xo{s}"], writes=["x1"])
                T.dma("sp", X1[k * 128:(k + 1) * 128, :], x1[:], reads=["x1"], key="st_x1")
                norm_T2(x1, "x1")
                T.dma("sp", H2T[:, :, k * 128:(k + 1) * 128], hT[:], reads=["hTd"], key="st_hTd")
                for q4 in range(4):
                    bank = SB_[1 + (q4 % 2)]
                    bk = f"S{1 + (q4 % 2)}"
                    for a in range(4):
                        hp = q4 * 4 + a
                        for c in range(8):
                            T.op("pe", lambda e, c=c, hp=hp, a=a, bank=bank: e.matmul(
                                bank[:, a * 128:(a + 1) * 128], lhsT=w_pq_sb[:, c, hp * 128:(hp + 1) * 128], rhs=hT[:, c, :],
                                start=(c == 0), stop=(c == 7)), reads=["w_pq_sb", "hTd"], writes=[bk])
                    T.op("act", lambda e, q4=q4, bank=bank: e.activation(
                        out=qpT[:, q4 * 4:(q4 + 1) * 4, :], in_=bank[:, :].rearrange("p (a t) -> p a t", t=128), func=AF.Copy),
                        reads=[bk], writes=["qpT"])
                for hp in range(16):
                    T.op("pe", lambda e, hp=hp: e.matmul(scp[:, hp * 128:(hp + 1) * 128], lhsT=qpT[:, hp, :], rhs=skT_sb[:, hp, :],
                                                         start=True, stop=True), reads=["qpT", "skT_sb"], writes=["P4"])
                T.op("act", lambda e: e.activation(out=scs2[s][:], in_=scp[:, :].rearrange("p (a n) -> p a n", n=128), func=AF.Copy),
                     reads=["P4"], writes=[f"scs{s}"])

            def d_tail(k):
                s = k % 2
                scs = scs2[s]
                top16(scs, f"scs{s}", scw, "scw", 16, 128, top, "top", tix, "tix")
                T.op("dve", lambda e: e.tensor_copy(out=tixf[:], in_=tix[:]), reads=["tix"], writes=["tixf"])
                tv = top[:].rearrange("p (h two) a -> p h two a", two=2)
                s1t = tv[:, :, 0, :]
                s2t = tv[:, :, 1, :]
                c4 = cand[:].rearrange("p h (a b) -> p h a b", b=16)
                T.op("dve", lambda e: e.tensor_tensor(out=c4, in0=s1t.unsqueeze(3).to_broadcast([128, 8, 16, 16]),
                                                      in1=s2t.unsqueeze(2).to_broadcast([128, 8, 16, 16]), op=ALU.add),
                     reads=["top"], writes=["cand"])
                top16(cand, "cand", candw, "candw", 8, 256, ts, "ts", tc, "tc")
                T.op("dve", lambda e: e.tensor_single_scalar(out=tca[:], in_=tc[:], scalar=4, op=ALU.arith_shift_right),
                     reads=["tc"], writes=["tca"])
                T.op("dve", lambda e: e.tensor_single_scalar(out=tcb[:], in_=tc[:], scalar=15, op=ALU.bitwise_and),
                     reads=["tc"], writes=["tcb"])
                T.op("dve", lambda e: e.tensor_copy(out=taf[:], in_=tca[:]), reads=["tca"], writes=["taf"])
                T.op("dve", lambda e: e.tensor_copy(out=tbf[:], in_=tcb[:]), reads=["tcb"], writes=["tbf"])
                iv = tixf[:].rearrange("p (h two) a -> p h two a", two=2)
                io16 = iota[:, 0:16].unsqueeze(1).unsqueeze(1).to_broadcast([128, 8, 16, 16])
                ee = ees[s]
                eek = f"ee{s}"
                for (sel, selk, which) in ((taf, "taf", 0), (tbf, "tbf", 1)):
                    oh = ohs[s][which]
                    ohk = f"oh{s}{which}"
                    T.op("dve", lambda e: e.tensor_tensor(out=oh[:], in0=sel[:].unsqueeze(3).to_broadcast([128, 8, 16, 16]),
                                                          in1=io16, op=ALU.is_equal), reads=[selk, "iota"], writes=[ohk])
                    T.op("dve", lambda e: e.tensor_tensor(
                        out=oh[:], in0=oh[:], in1=iv[:, :, which, :].unsqueeze(2).to_broadcast([128, 8, 16, 16]), op=ALU.mult),
                        reads=[ohk, "tixf"], writes=[ohk])
                g3 = ee[:, 2, :].rearrange("p (h k) -> p h k", k=16)
                T.op("dve", lambda e: e.tensor_tensor(out=g3, in0=ts[:], in1=ts[:, :, 0:1].to_broadcast([128, 8, 16]), op=ALU.subtract),
                     reads=["ts"], writes=[eek])
                T.op("act", lambda e: e.activation(out=g3, in_=g3, func=AF.Exp), reads=[eek], writes=[eek])
                T.op("dve", lambda e: e.tensor_reduce(out=gsum[:], in_=g3, axis=AX.X, op=ALU.add), reads=[eek], writes=["gsum"])
                T.op("dve", lambda e: e.reciprocal(out=gsum[:], in_=gsum[:]), reads=["gsum"], writes=["gsum"])
                T.op("dve", lambda e: e.tensor_tensor(out=g3, in0=g3, in1=gsum[:].unsqueeze(2).to_broadcast([128, 8, 16]), op=ALU.mult),
                     reads=[eek, "gsum"], writes=[eek])

            def d_tail2(k):
                s = k % 2
                ee = ees[s]
                eek = f"ee{s}"
                for which in range(2):
                    T.op("dve", lambda e: e.tensor_reduce(out=ee[:, which, :].rearrange("p (h k) -> p h k", k=16), in_=ohs[s][which][:],
                                                          axis=AX.X, op=ALU.add), reads=[f"oh{s}{which}"], writes=[eek])
                for a in range(3):
                    T.op("pe", lambda e: e.transpose(out=SB_[3][:, a * 128:(a + 1) * 128], in_=ee[:, a, :], identity=ident_f[:]),
                         reads=[eek, "ident_f"], writes=["S3"])
                T.op("act", lambda e: e.activation(out=eT[:], in_=SB_[3][:, 0:384].rearrange("p (a t) -> p a t", t=128), func=AF.Copy),
                     reads=["S3"], writes=["eT"])
                T.dma("sp", ET[:, :, k * 128:(k + 1) * 128].rearrange("a p t -> p a t"), eT[:], reads=["eT"], key="st_eT")

            d_load(0)
            for k in range(NO):
                if k + 1 < NO:
                    d_load(k + 1)
                d_front(k)
                if k > 0:
                    d_tail(k - 1)
                if k > 1:
                    d_tail2(k - 2)
            d_tail(NO - 1)
            d_tail2(NO - 2)
            d_tail2(NO - 1)
            T.drain()

        if STOP_AFTER == "D":
            return nc

        with ExitStack() as pe_:
            TG = 256
            NG = SO // TG
            CH = 4
            NCH = TG // CH
            SBK = 4
            NSB = 128 // SBK
            Gs = [sb(pe_, [128, TG, 128], BF16, f"G{i}") for i in range(2)]
            NSL = 2
            Us = [sb(pe_, [128, SBK, 1024], BF16, f"Us{i}") for i in range(NSL)]
            Vs2 = [sb(pe_, [128, SBK, 1024], BF16, f"Vs2{i}") for i in range(NSL)]
            h2gs = [sb(pe_, [128, 8, TG], BF16, f"h2g{i}") for i in range(2)]
            etgs = [sb(pe_, [128, 3, TG], F32, f"etg{i}") for i in range(2)]
            x1g = sb(pe_, [128, D], F32, "x1g")
            og = sb(pe_, [128, D], F32, "og")
            E1 = [sb(pe_, [128, CH, 128], BF16, f"E1{i}") for i in range(2)]
            E2 = [sb(pe_, [128, CH, 128], BF16, f"E2{i}") for i in range(2)]
            ga = [sb(pe_, [128, TG], BF16, f"ga{i}") for i in range(4)]
            wT = [sb(pe_, [128, TG], BF16, f"wT{i}") for i in range(4)]
            yps = P4
            aps = [SB_[0], SB_[1], SB_[2]]
            gpb = SB_[3]
            Uv = Ubf.rearrange("(i p) (c j) -> p i (c j)", p=128, j=128)
            Vv = Vbf.rearrange("(i j) d -> j i d", j=128)
            io3 = iota[:].unsqueeze(1).to_broadcast([128, CH, 128])
            cctr = [0]

            TOT = NG * NSB

            def load_u(gsb):
                if gsb >= TOT:
                    return
                s, sbi = gsb % NSL, gsb % NSB
                T.dma("sp", Us[s][:], Uv[:, sbi * SBK:(sbi + 1) * SBK, :], writes=[f"Us{s}"], key=f"ld_Us{s}")

            def load_v(gsb):
                if gsb >= TOT:
                    return
                s, sbi = gsb % NSL, gsb % NSB
                T.dma("act", Vs2[s][:], Vv[:, sbi * SBK:(sbi + 1) * SBK, :], writes=[f"Vs2{s}"], key=f"ld_Vs2{s}")

            def load_group(gi):
                t0 = gi * TG
                T.dma("sp", h2gs[gi % 2][:], H2T[:, :, t0:t0 + TG], writes=[f"h2g{gi % 2}"], key=f"ld_h2g{gi % 2}")
                T.dma("sp", etgs[gi % 2][:], ET[:, :, t0:t0 + TG].rearrange("a p t -> p a t"), writes=[f"etg{gi % 2}"],
                      key=f"ld_etg{gi % 2}")

            def gb_prep(gi, ci):
                etg = etgs[gi % 2]
                ek = f"etg{gi % 2}"
                cs_ = ci % 2
                tt = slice(ci * CH, (ci + 1) * CH)
                e1b = etg[:, 0, tt].unsqueeze(2).to_broadcast([128, CH, 128])
                e2b = etg[:, 1, tt].unsqueeze(2).to_broadcast([128, CH, 128])
                gb = etg[:, 2, tt].unsqueeze(2).to_broadcast([128, CH, 128])
                T.op("dve", lambda e: e.tensor_tensor(out=E1[cs_][:], in0=io3, in1=e1b, op=ALU.is_equal),
                     reads=["iota", ek], writes=[f"E1{cs_}"])
                T.op("pool", lambda e: e.tensor_tensor(out=E1[cs_][:], in0=E1[cs_][:], in1=gb, op=ALU.mult),
                     reads=[f"E1{cs_}", ek], writes=[f"E1{cs_}"])
                T.op("dve", lambda e: e.tensor_tensor(out=E2[cs_][:], in0=io3, in1=e2b, op=ALU.is_equal),
                     reads=["iota", ek], writes=[f"E2{cs_}"])

            def gb_mm(gi, ci):
                G = Gs[gi % 2]
                Gk = f"G{gi % 2}"
                cs_ = ci % 2
                for a in range(4):
                    T.op("pe", lambda e: e.matmul(gpb[:, a * 128:(a + 1) * 128], lhsT=E2[cs_][:, a, :],
                                                  rhs=E1[cs_][:, a, :], start=True, stop=True),
                         reads=[f"E1{cs_}", f"E2{cs_}"], writes=["S3"])
                tb = ci * CH
                T.op("act", lambda e: e.activation(out=G[:, tb:tb + 4, :], in_=gpb[:, :].rearrange("p (a i) -> p a i", i=128),
                                                   func=AF.Copy), reads=["S3"], writes=[Gk])

            LOOK = 2

            def emit_A(gi, n):
                sbi, il = divmod(n, SBK)
                s = (gi * NSB + sbi) % NSL
                ap_ = aps[n % 3]
                apk = f"S{n % 3}"
                h2g = h2gs[gi % 2]
                for c in range(8):
                    T.op("pe", lambda e: e.matmul(ap_[:, 0:TG], lhsT=Us[s][:, il, c * 128:(c + 1) * 128],
                                                  rhs=h2g[:, c, :], start=(c == 0), stop=(c == 7)),
                         reads=[f"Us{s}", f"h2g{gi % 2}"], writes=[apk])
                T.op("act", lambda e: e.activation(out=ga[n % 4][:], in_=ap_[:, 0:TG], func=AF.Gelu_apprx_tanh),
                     reads=[apk], writes=[f"ga{n % 4}"])
                weng = "dve" if n % 2 == 0 else "pool"
                T.op(weng, lambda e: e.tensor_tensor(out=wT[n % 4][:], in0=ga[n % 4][:], in1=Gs[gi % 2][:, :, n], op=ALU.mult),
                     reads=[f"ga{n % 4}", f"G{gi % 2}"], writes=[f"wT{n % 4}"])

            def emit_Y(gi, n):
                sbi, il = divmod(n, SBK)
                s = (gi * NSB + sbi) % NSL
                for a in range(2):
                    for half in range(2):
                        T.op("pe", lambda e: e.matmul(
                            yps[:, (a * 2 + half) * 512:(a * 2 + half + 1) * 512], lhsT=wT[n % 4][:, a * 128:(a + 1) * 128],
                            rhs=Vs2[s][:, il, half * 512:(half + 1) * 512], start=(n == 0), stop=(n == 127)),
                            reads=[f"wT{n % 4}", f"Vs2{s}"], writes=["P4"])

            load_group(0)
            for q in range(NSL):
                load_u(q)
                load_v(q)
            gb_prep(0, 0)
            for ci in range(NCH):
                if ci + 1 < NCH:
                    gb_prep(0, ci + 1)
                gb_mm(0, ci)
            for gi in range(NG):
                t0 = gi * TG
                if gi + 1 < NG:
                    load_group(gi + 1)
                    gb_prep(gi + 1, 0)
                for n in range(128 + LOOK):
                    if n < 128:
                        emit_A(gi, n)
                        if n % SBK == SBK - 1:
                            load_u(gi * NSB + n // SBK + NSL)
                        if gi + 1 < NG and n % 2 == 1:
                            ci = n // 2
                            if ci + 1 < NCH:
                                gb_prep(gi + 1, ci + 1)
                            gb_mm(gi + 1, ci)
                    m = n - LOOK
                    if m >= 0:
                        emit_Y(gi, m)
                        if m % SBK == SBK - 1:
                            load_v(gi * NSB + m // SBK + NSL)
                for a in range(2):
                    T.dma("sp", x1g[:], X1[t0 + a * 128:t0 + (a + 1) * 128, :], writes=["x1g"], key="ld_x1g")
                    T.op("dve", lambda e: e.tensor_tensor(out=og[:], in0=yps[:, a * 1024:(a + 1) * 1024], in1=gate2[:], op=ALU.mult),
                         reads=["P4", "gate2"], writes=["og"])
                    T.op("pool", lambda e: e.tensor_tensor(out=og[:], in0=og[:], in1=x1g[:], op=ALU.add),
                         reads=["og", "x1g"], writes=["og"])
                    T.dma("sp", out_own[t0 + a * 128:t0 + (a + 1) * 128, :], og[:], reads=["og"], key="st_og")
            T.drain()
    return nc


def _host_inputs(inp):
    f32 = np.float32
    x = np.asarray(inp["x"], f32)
    c = np.asarray(inp["c"], f32)
    pos = np.asarray(inp["positions"], np.int32)
    U = np.asarray(inp["peer_u"], f32)
    U_arr = np.ascontiguousarray(U.reshape(128, 128, 8, 128).transpose(0, 3, 2, 1)).reshape(16384, 1024)
    sk = np.asarray(inp["sub_keys"], f32)
    skT = np.ascontiguousarray(sk.reshape(16, 128, 128).transpose(2, 0, 1)).reshape(128, 16 * 128)
    fm = lambda v, n: np.ascontiguousarray(np.asarray(v, f32).reshape(n, 128).T)
    shared = {
        "w_ada": np.asarray(inp["w_ada"], f32),
        "bada_fm": fm(inp["b_ada"], 48),
        "bada_row": np.asarray(inp["b_ada"], f32).reshape(1, -1),
        "g1_fm": fm(inp["norm1_g"], 8), "g2_fm": fm(inp["norm2_g"], 8),
        "w_in": np.asarray(inp["w_in"], f32),
        "gqa_fm": fm(inp["mla_qa_g"], 3), "gkva_fm": fm(inp["mla_kva_g"], 2),
        "w_uq": np.asarray(inp["w_uq"], f32), "w_ukv": np.asarray(inp["w_ukv"], f32),
        "gq_row": np.asarray(inp["mla_q_g"], f32).reshape(1, -1), "gk_row": np.asarray(inp["mla_k_g"], f32).reshape(1, -1),
        "gfq_row": np.asarray(inp["fox_q_g"], f32).reshape(1, -1), "gfk_row": np.asarray(inp["fox_k_g"], f32).reshape(1, -1),
        "bf_col": np.asarray(inp["b_f"], f32).reshape(8, 1),
        "w_o": np.asarray(inp["w_o"], f32), "w_pq": np.asarray(inp["w_pq"], f32),
        "skT": skT, "U_arr": U_arr, "V_in": np.asarray(inp["peer_v"], f32),
        "c_ident_bf": np.eye(128, dtype=f32).astype(ml_dtypes.bfloat16),
        "c_ident_f": np.eye(128, dtype=f32),
        "c_invf": np.tile((f32(10000.0) ** (-np.arange(0, 32, 2, dtype=f32) / f32(32))).astype(f32)[None, :], (128, 1)),
        "c_iota": np.tile(np.arange(128, dtype=f32)[None, :], (128, 1)),
    }
    r = np.arange(128)
    tri = np.where(r[:, None] > r[None, :], NEG, 0.0).astype(f32)
    full = np.full((128, 128), NEG, f32)
    zero = np.zeros((128, 128), f32)
    in_maps = []
    for core in range(8):
        b, par = core // 2, core % 2
        xb = x[b]
        x_own = np.ascontiguousarray(xb.reshape(32, 2, 128, D)[:, par]).reshape(SO, D)
        pb = pos[b]
        m = dict(shared)
        m.update({
            "x_all": xb, "x_own": x_own,
            "cT": np.ascontiguousarray(c[b].reshape(8, 128).T),
            "pos_all": np.ascontiguousarray(pb.reshape(64, 128).T),
            "pos_own": np.ascontiguousarray(pb.reshape(32, 2, 128)[:, par].T),
            "c_maskA": (tri if par == 0 else zero).astype(ml_dtypes.bfloat16),
            "c_maskB": (full if par == 0 else tri).astype(ml_dtypes.bfloat16),
            "c_par": np.full((128, 1), float(par), f32),
        })
        in_maps.append(m)
    return in_maps


def kernel(**inputs):
    in_maps = _host_inputs(inputs)
    nc = build_program()
    res = run_bass_kernel_spmd(nc, in_maps, core_ids=list(range(8)))
    out = np.empty((4, S, D), np.float32)
    for core in range(8):
        b, par = core // 2, core % 2
        o = np.asarray(res.results[core]["out_own"], np.float32).reshape(32, 128, D)
        out[b].reshape(32, 2, 128, D)[:, par] = o
    return out
```
